# Optimizing a Trainium2 kernel written in Bass

```python
import math
import jax
import jax.numpy as jnp
from jax import lax
import numpy as np

D_MODEL = 1024
BATCH = 8
SEQ = 2048
DEPTH = 1

HEAD_DIM = 64
MIX_WIDTH = D_MODEL
A_WIDTH = MIX_WIDTH // 2
B_WIDTH = MIX_WIDTH - A_WIDTH
A_HEADS = A_WIDTH // HEAD_DIM
B_HEADS = B_WIDTH // (2 * HEAD_DIM)
DIL_PATTERNS = ((128, 1), (512, 4), (2048, 16))
BAND_BLOCK = 128
Q_BLOCK = 128
N_EXPERTS = 32
TOP_K = 4
D_FF = D_MODEL
SWIGLU_LIMIT = 7.0
SWIGLU_ALPHA = 1.702
MOE_BLOCK = 128
RMS_EPS = 1e-6
ATTN_SCALE = HEAD_DIM ** -0.5
IN_SIZES = (A_WIDTH, A_WIDTH, A_WIDTH, B_WIDTH, B_WIDTH, B_WIDTH)
IN_WIDTH = sum(IN_SIZES)
IN_SPLITS = tuple(int(v) for v in np.cumsum(IN_SIZES)[:-1])

kernel_name = 'hymba_style_dilated_diffattn_moe_block'


def rms_norm(x, g):
    xf = x.astype(jnp.float32)
    y = xf * lax.rsqrt(jnp.mean(xf * xf, axis=-1, keepdims=True) + RMS_EPS)
    return (y * g.astype(jnp.float32)).astype(x.dtype)


def alibi_slopes(n):
    return 2.0 ** (-8.0 * jnp.arange(1, n + 1, dtype=jnp.float32) / n)


def dilated_band_attention(q, k, v, slopes, window, dil):
    bsz, nh, n_tok, hd = q.shape
    L = n_tok // dil
    w_sub = window // dil
    nb = -(-L // BAND_BLOCK)
    Lp = nb * BAND_BLOCK

    def to_sub(t):
        t = jnp.swapaxes(t.reshape(bsz, nh, L, dil, hd), 2, 3)
        return jnp.pad(t, ((0, 0), (0, 0), (0, 0), (0, Lp - L), (0, 0)))

    def key_band(t):
        t = jnp.pad(to_sub(t), ((0, 0), (0, 0), (0, 0), (BAND_BLOCK, 0), (0, 0)))
        t = t.reshape(bsz, nh, dil, nb + 1, BAND_BLOCK, hd)
        return jnp.concatenate([t[:, :, :, :-1], t[:, :, :, 1:]], axis=4)

    qs = to_sub(q).reshape(bsz, nh, dil, nb, BAND_BLOCK, hd)
    ks, vs = key_band(k), key_band(v)
    iq = jnp.arange(BAND_BLOCK)[:, None]
    ik = jnp.arange(2 * BAND_BLOCK)[None, :]
    delta = iq - ik + BAND_BLOCK
    k_sub = (jnp.arange(nb)[:, None] * BAND_BLOCK - BAND_BLOCK
             + jnp.arange(2 * BAND_BLOCK)[None, :])
    valid = ((delta >= 0) & (delta <= w_sub))[None] & (k_sub >= 0)[:, None, :]
    bias = -slopes[:, None, None] * (delta * dil).astype(jnp.float32)[None]
    s = jnp.einsum('bhrnqd,bhrnkd->bhrnqk', qs, ks).astype(jnp.float32) * ATTN_SCALE
    s = s + bias[None, :, None, None]
    s = jnp.where(valid[None, None, None], s, -jnp.inf)
    m = jnp.max(s, axis=-1, keepdims=True)
    e = jnp.exp(s - m)
    den = jnp.sum(e, axis=-1, keepdims=True)
    o = jnp.einsum('bhrnqk,bhrnkd->bhrnqd', (e / den).astype(v.dtype), vs)
    lse = (m + jnp.log(den))[..., 0]

    def from_sub(t):
        tail = t.shape[5:]
        t = t.reshape((bsz, nh, dil, Lp) + tail)[:, :, :, :L]
        t = jnp.swapaxes(t, 2, 3)
        return t.reshape((bsz, nh, n_tok) + tail)

    return from_sub(o), from_sub(lse)


def dilated_mixture_attention(q, k, v, slopes):
    outs, lses = [], []
    for window, dil in DIL_PATTERNS:
        o, lse = dilated_band_attention(q, k, v, slopes, window, dil)
        outs.append(o)
        lses.append(lse)
    w = jax.nn.softmax(jnp.stack(lses, axis=0), axis=0)
    return jnp.einsum('pbhs,pbhsd->bhsd', w.astype(q.dtype), jnp.stack(outs, axis=0))


def differential_attention(q, k, v, slopes, lam):
    bsz, nh, n_tok, _, hd = q.shape
    nqb = n_tok // Q_BLOCK
    qb = jnp.moveaxis(q.reshape(bsz, nh, nqb, Q_BLOCK, 2, hd), 2, 0)
    k_pos = jnp.arange(n_tok)

    def block(args):
        q_blk, start = args
        dist = (start + jnp.arange(Q_BLOCK))[:, None] - k_pos[None, :]
        sc = jnp.einsum('bhqcd,bhkcd->bhcqk', q_blk, k).astype(jnp.float32) * ATTN_SCALE
        sc = sc - slopes[None, :, None, None, None] * dist.astype(jnp.float32)
        sc = jnp.where(dist >= 0, sc, -jnp.inf)
        p = jax.nn.softmax(sc, axis=-1)
        a = p[:, :, 0] - lam * p[:, :, 1]
        return jnp.einsum('bhqk,bhkd->bhqd', a.astype(v.dtype), v)

    o = lax.map(block, (qb, jnp.arange(nqb) * Q_BLOCK))
    return jnp.moveaxis(o, 0, 2).reshape(bsz, nh, n_tok, 2 * hd)


def moe_ffn(h, w_router, b_router, w_gate_up, b_gate_up, w_down, b_down):
    n, d = h.shape
    logits = (h @ w_router).astype(jnp.float32) + b_router.astype(jnp.float32)
    top_val, top_idx = lax.top_k(logits, TOP_K)
    gates = jax.nn.softmax(top_val, axis=-1)
    n_assign = n * TOP_K
    flat_e = top_idx.reshape(-1)
    order = jnp.argsort(flat_e)
    e_sorted = flat_e[order]
    tok_sorted = order // TOP_K
    gate_sorted = gates.reshape(-1)[order]
    counts = jnp.zeros((N_EXPERTS,), jnp.int32).at[flat_e].add(1)
    padded = (counts + MOE_BLOCK - 1) // MOE_BLOCK * MOE_BLOCK
    pad_end = jnp.cumsum(padded)
    pad_start = pad_end - padded
    grp_start = jnp.cumsum(counts) - counts
    dest = pad_start[e_sorted] + jnp.arange(n_assign) - grp_start[e_sorted]
    n_blocks = -(-(n_assign + N_EXPERTS * (MOE_BLOCK - 1)) // MOE_BLOCK)
    buf_tok = jnp.zeros((n_blocks * MOE_BLOCK,), jnp.int32).at[dest].set(tok_sorted)
    block_e = jnp.minimum(jnp.searchsorted(pad_end, jnp.arange(n_blocks) * MOE_BLOCK, side='right'),
                          N_EXPERTS - 1)
    xb = h[buf_tok].reshape(n_blocks, MOE_BLOCK, d)

    def expert_block(args):
        xe, e = args
        gu = xe @ w_gate_up[e] + b_gate_up[e]
        g = jnp.minimum(gu[:, :D_FF], SWIGLU_LIMIT)
        u = jnp.clip(gu[:, D_FF:], -SWIGLU_LIMIT, SWIGLU_LIMIT)
        act = (u + 1.0) * (g * jax.nn.sigmoid(SWIGLU_ALPHA * g))
        return act @ w_down[e] + b_down[e]

    yb = lax.map(expert_block, (xb, block_e)).reshape(-1, d)
    y = yb[dest] * gate_sorted[:, None].astype(yb.dtype)
    return jax.ops.segment_sum(y, tok_sorted, num_segments=n)


def hybrid_layer(x, c, w_ada, b_ada, norm1_g, w_in, a_q_norm_g, a_k_norm_g, b_q_norm_g,
                 b_k_norm_g, lambda_q1, lambda_k1, lambda_q2, lambda_k2, diff_norm_g, w_out,
                 norm2_g, w_router, b_router, w_gate_up, b_gate_up, w_down, b_down, lambda_init):
    bsz, n_tok, d = x.shape
    mod = jax.nn.silu(c) @ w_ada + b_ada
    shift1, scale1, gate1, shift2, scale2, gate2 = jnp.split(mod[:, None, :], 6, axis=-1)

    h = rms_norm(x, norm1_g) * (1.0 + scale1) + shift1
    proj = h @ w_in
    aq, ak, av, bq, bk, bv = jnp.split(proj, IN_SPLITS, axis=-1)

    def heads_a(t):
        return jnp.swapaxes(t.reshape(bsz, n_tok, A_HEADS, HEAD_DIM), 1, 2)

    aq = rms_norm(heads_a(aq), a_q_norm_g)
    ak = rms_norm(heads_a(ak), a_k_norm_g)
    o_a = dilated_mixture_attention(aq, ak, heads_a(av), alibi_slopes(A_HEADS))
    o_a = jnp.swapaxes(o_a, 1, 2).reshape(bsz, n_tok, A_WIDTH)

    def heads_b(t):
        return jnp.swapaxes(t.reshape(bsz, n_tok, B_HEADS, 2, HEAD_DIM), 1, 2)

    bq = rms_norm(heads_b(bq), b_q_norm_g)
    bk = rms_norm(heads_b(bk), b_k_norm_g)
    bv = jnp.swapaxes(bv.reshape(bsz, n_tok, B_HEADS, 2 * HEAD_DIM), 1, 2)
    lam = (jnp.exp(jnp.sum(lambda_q1.astype(jnp.float32) * lambda_k1.astype(jnp.float32)))
           - jnp.exp(jnp.sum(lambda_q2.astype(jnp.float32) * lambda_k2.astype(jnp.float32)))
           + lambda_init)
    o_b = differential_attention(bq, bk, bv, alibi_slopes(B_HEADS), lam)
    o_b = rms_norm(o_b, diff_norm_g) * (1.0 - lambda_init)
    o_b = jnp.swapaxes(o_b, 1, 2).reshape(bsz, n_tok, B_WIDTH)

    mixed = jnp.concatenate([o_a, o_b], axis=-1) @ w_out
    x = x + gate1 * mixed

    h2 = rms_norm(x, norm2_g) * (1.0 + scale2) + shift2
    y = moe_ffn(h2.reshape(bsz * n_tok, d), w_router, b_router, w_gate_up, b_gate_up,
                w_down, b_down).reshape(bsz, n_tok, d)
    return x + gate2 * y


def setup_inputs(seed: int = 0) -> dict:
    key = jax.random.key(seed)
    ks = jax.random.split(key, 24)

    def nrm(k, shape, scale):
        return jax.random.normal(k, shape, jnp.float32) * scale

    def gain(k, shape):
        return 1.0 + nrm(k, shape, 0.05)

    L = DEPTH
    return {
        'x': nrm(ks[0], (BATCH, SEQ, D_MODEL), 1.0),
        'c': nrm(ks[1], (BATCH, D_MODEL), 1.0),
        'w_ada': nrm(ks[2], (L, D_MODEL, 6 * D_MODEL), 0.5 * D_MODEL ** -0.5),
        'b_ada': nrm(ks[3], (L, 6 * D_MODEL), 0.02),
        'norm1_g': gain(ks[4], (L, D_MODEL)),
        'w_in': nrm(ks[5], (L, D_MODEL, IN_WIDTH), D_MODEL ** -0.5),
        'a_q_norm_g': gain(ks[6], (L, HEAD_DIM)),
        'a_k_norm_g': gain(ks[7], (L, HEAD_DIM)),
        'b_q_norm_g': gain(ks[8], (L, HEAD_DIM)),
        'b_k_norm_g': gain(ks[9], (L, HEAD_DIM)),
        'lambda_q1': nrm(ks[10], (L, HEAD_DIM), 0.1),
        'lambda_k1': nrm(ks[11], (L, HEAD_DIM), 0.1),
        'lambda_q2': nrm(ks[12], (L, HEAD_DIM), 0.1),
        'lambda_k2': nrm(ks[13], (L, HEAD_DIM), 0.1),
        'diff_norm_g': gain(ks[14], (L, 2 * HEAD_DIM)),
        'w_out': nrm(ks[15], (L, MIX_WIDTH, D_MODEL), MIX_WIDTH ** -0.5),
        'norm2_g': gain(ks[16], (L, D_MODEL)),
        'w_router': nrm(ks[17], (L, D_MODEL, N_EXPERTS), D_MODEL ** -0.5),
        'b_router': nrm(ks[18], (L, N_EXPERTS), 0.01),
        'w_gate_up': nrm(ks[19], (L, N_EXPERTS, D_MODEL, 2 * D_FF), D_MODEL ** -0.5),
        'b_gate_up': nrm(ks[20], (L, N_EXPERTS, 2 * D_FF), 0.01),
        'w_down': nrm(ks[21], (L, N_EXPERTS, D_FF, D_MODEL), D_FF ** -0.5),
        'b_down': nrm(ks[22], (L, N_EXPERTS, D_MODEL), 0.01),
    }


def reference(x, c, w_ada, b_ada, norm1_g, w_in, a_q_norm_g, a_k_norm_g, b_q_norm_g, b_k_norm_g,
              lambda_q1, lambda_k1, lambda_q2, lambda_k2, diff_norm_g, w_out, norm2_g, w_router,
              b_router, w_gate_up, b_gate_up, w_down, b_down):
    for l in range(DEPTH):
        lambda_init = 0.8 - 0.6 * math.exp(-0.3 * l)
        x = hybrid_layer(x, c, w_ada[l], b_ada[l], norm1_g[l], w_in[l], a_q_norm_g[l],
                         a_k_norm_g[l], b_q_norm_g[l], b_k_norm_g[l], lambda_q1[l], lambda_k1[l],
                         lambda_q2[l], lambda_k2[l], diff_norm_g[l], w_out[l], norm2_g[l],
                         w_router[l], b_router[l], w_gate_up[l], b_gate_up[l], w_down[l],
                         b_down[l], lambda_init)
    return x
```

```python
import numpy as np
from contextlib import ExitStack
import concourse.bass as bass
import concourse.mybir as mybir
from concourse.bass_utils import run_bass_kernel_spmd

F32 = mybir.dt.float32
BF16 = mybir.dt.bfloat16
ALU = mybir.AluOpType
AF = mybir.ActivationFunctionType
AX = mybir.AxisListType

S = 2048
D = 1024
NT = 16
NE = 32
EPS = 1e-6
KB = 1024
ARENA = 206 * KB
SLOPES_A = [2.0 ** (-8.0 * (i + 1) / 8) for i in range(8)]
SLOPES_B = [2.0 ** (-8.0 * (i + 1) / 4) for i in range(4)]
LAM_INIT = 0.8 - 0.6 * 1.0
BIG = 1.0e6

_SM = {}
_off = 0
for _n, _w in [("c", 8), ("bada", 48), ("g1", 8), ("g2", 8), ("gqa", 64), ("gka", 64), ("gqb", 64),
               ("gkb", 64), ("lam", 256), ("gdiff", 128), ("br", 32), ("bcolb", 76), ("bgu", 512),
               ("wr", 256), ("ident", 128), ("dcur", 128), ("dprev", 128), ("tri", 128),
               ("bg1", 1024), ("bg2", 1024)]:
    _SM[_n] = (_off, _w)
    _off += _w
NS = _off


class Buf:
    __slots__ = ("w", "r")

    def __init__(self):
        self.w = None
        self.r = {}


class Tracker:
    NDS = 24

    def __init__(self, nc, es):
        self.nc = nc
        self.eng = dict(pe=nc.tensor, act=nc.scalar, dve=nc.vector, pool=nc.gpsimd, sp=nc.sync)
        self.sem = {k: es.enter_context(nc.semaphore("s_" + k)) for k in ("pe", "act", "dve", "pool")}
        self.cnt = {k: 0 for k in self.sem}
        self.seen = {k: {} for k in self.eng}
        self.pending = {k: [] for k in self.eng}
        self.dsems = [es.enter_context(nc.semaphore("d%d" % i)) for i in range(self.NDS)]
        self.dcnt = [0] * self.NDS
        self.dnext = 0

    def wait(self, e, key, val):
        if self.seen[e].get(key, 0) >= val:
            return
        if key == ("c", "pe") and e == "pe":
            return
        sem = self.sem[key[1]] if key[0] == "c" else self.dsems[key[1]]
        self.eng[e].wait_ge(sem, val)
        self.seen[e][key] = val

    def _deps(self, e, r, w):
        for b in r:
            if b.w is not None:
                self.wait(e, *b.w)
        for b in w:
            if b.w is not None:
                self.wait(e, *b.w)
            for k, v in b.r.items():
                self.wait(e, k, v)

    def op(self, e, fn, r=(), w=(), sig=True):
        self._deps(e, r, w)
        ins = fn(self.eng[e])
        self.pending[e].append((r, w))
        if sig:
            self.cnt[e] += 1
            ins.then_inc(self.sem[e], 1)
            key = ("c", e)
            val = self.cnt[e]
            for (rr, ww) in self.pending[e]:
                for b in rr:
                    b.r[key] = val
                for b in ww:
                    b.w = (key, val)
                    b.r = {}
            self.pending[e] = []
        return ins

    def dma(self, out, in_, r=(), w=(), q="sp"):
        self._deps(q, r, w)
        i = self.dnext
        self.dnext = (i + 1) % self.NDS
        key = ("d", i)
        if self.dcnt[i] > 0:
            self.wait(q, key, self.dcnt[i])
        self.dcnt[i] += 16
        self.eng[q].dma_start(out=out, in_=in_).then_inc(self.dsems[i], 16)
        val = self.dcnt[i]
        for b in r:
            b.r[key] = val
        for b in w:
            b.w = (key, val)
            b.r = {}

    def barrier(self):
        for e in ("pe", "act", "dve", "pool", "sp"):
            for k in ("pe", "act", "dve", "pool"):
                if self.cnt[k] > 0 and k != e:
                    self.wait(e, ("c", k), self.cnt[k])
            for i in range(self.NDS):
                if self.dcnt[i] > 0:
                    self.wait(e, ("d", i), self.dcnt[i])


def _dsize(dt):
    return 2 if dt == BF16 else 4


def build_nc(debug=False):
    nc = bass.Bass("TRN2", target_bir_lowering=False)
    x_d = nc.dram_tensor("x", [S, D], F32, kind="ExternalInput").ap()
    sm_d = nc.dram_tensor("smalls", [128, NS], F32, kind="ExternalInput").ap()
    wada_d = nc.dram_tensor("w_ada", [D, 6 * D], F32, kind="ExternalInput").ap()
    win_d = nc.dram_tensor("w_in", [D, 3 * D], F32, kind="ExternalInput").ap()
    wout_d = nc.dram_tensor("w_out", [D, D], F32, kind="ExternalInput").ap()
    wgu_d = nc.dram_tensor("w_gate_up", [NE, D, 2 * D], F32, kind="ExternalInput").ap()
    wd_d = nc.dram_tensor("w_down", [NE, D, D], F32, kind="ExternalInput").ap()
    bd_d = nc.dram_tensor("b_down", [NE, D], F32, kind="ExternalInput").ap()
    out_d = nc.dram_tensor("out", [S, D], F32, kind="ExternalOutput").ap()
    if debug:
        dbg_hT = nc.dram_tensor("dbg_hT", [128, 8, S], BF16, kind="ExternalOutput").ap()
        dbg_catT = nc.dram_tensor("dbg_catT", [128, 8, S], BF16, kind="ExternalOutput").ap()
        dbg_h2T = nc.dram_tensor("dbg_h2T", [128, 8, S], BF16, kind="ExternalOutput").ap()
        dbg_x1 = nc.dram_tensor("dbg_x1", [128, NT, D], F32, kind="ExternalOutput").ap()
        dbg_sm = nc.dram_tensor("dbg_sm", [128, 28 * KB // 4], F32, kind="ExternalOutput").ap()
        dbg_qk = nc.dram_tensor("dbg_qk", [128, 4, 4, S], BF16, kind="ExternalOutput").ap()

    with ExitStack() as es:
        arena = es.enter_context(nc.sbuf_tensor("arena", [128, ARENA // 4], F32))
        psum = es.enter_context(nc.psum_tensor("psum", [128, 8, 512], F32))
        T = Tracker(nc, es)
        _sc = [None]

        def scope(name):
            if _sc[0] is not None:
                nc.leave_named_scope(_sc[0][0], _sc[0][1], False)
            _sc[0] = None
            if name is not None:
                sid, _ = nc.enter_named_scope(name, False)
                _sc[0] = (name, sid)
        PB = [Buf() for _ in range(8)]

        def carve(off, shape, dt=F32, parts=128):
            n = 1
            for s_ in shape:
                n *= s_
            nb = n * _dsize(dt)
            assert off % 4 == 0 and nb % 4 == 0 and off + nb <= ARENA, (off, nb)
            a = arena[0:parts, off // 4:(off + nb) // 4]
            if dt != F32:
                a = a.bitcast(dt)
            if len(shape) == 2:
                a = a.rearrange("p (a b) -> p a b", a=shape[0])
            elif len(shape) == 3:
                a = a.rearrange("p (a b c) -> p a b c", a=shape[0], b=shape[1])
            return a

        class Region:
            def __init__(self, start, end):
                self.p = start
                self.end = end

            def alloc(self, shape, dt=F32, parts=128):
                n = 1
                for s_ in shape:
                    n *= s_
                nb = (n * _dsize(dt) + 3) // 4 * 4
                off = self.p
                self.p += nb
                assert self.p <= self.end, ("region overflow", self.p, self.end)
                return carve(off, shape, dt, parts)

        RP = Region(0, 28 * KB)
        smalls = RP.alloc([NS])
        SMB = Buf()

        def sm(name, lo=0, hi=None):
            o, w_ = _SM[name]
            hi = w_ if hi is None else hi
            return smalls[:, o + lo:o + hi]

        ident_bf = RP.alloc([128], BF16)
        tri_bf = RP.alloc([128], BF16)
        ones_bf = RP.alloc([64], BF16)
        siluc = RP.alloc([8])
        modc = RP.alloc([32])
        a1c = RP.alloc([8])
        a2c = RP.alloc([8])
        G_all = RP.alloc([NT, NE])
        GT = RP.alloc([S], BF16)
        bdp = RP.alloc([D], BF16)
        lamt = RP.alloc([8])
        stat = RP.alloc([64])
        CONST = Buf()
        MODB = Buf()
        GB1 = sm("bg1")
        GB2 = sm("bg2")
        ident_f = sm("ident")

        ACC0, HT0, R30 = 28 * KB, 92 * KB, 124 * KB
        acc = carve(ACC0, [NT, D])
        ACCB = [Buf() for _ in range(NT)]
        hT = carve(HT0, [8, S], BF16)
        HTB = Buf()
        catT = carve(R30, [8, S], BF16)
        CATB = Buf()

        T.dma(smalls, sm_d, w=[SMB])
        T.op("dve", lambda e: e.tensor_copy(out=ident_bf, in_=ident_f), r=[SMB], w=[CONST])
        T.op("dve", lambda e: e.tensor_copy(out=tri_bf, in_=sm("tri")), r=[SMB], w=[CONST])
        T.op("dve", lambda e: e.memset(ones_bf, 1.0), w=[CONST])

        scope('A')
        wst = [carve(ACC0, [8, D]), carve(ACC0 + 32 * KB, [8, D]), carve(R30, [8, D])]
        WSTB = [Buf(), Buf(), Buf()]
        sbc = carve(172 * KB, [8, 128])
        SBCB = Buf()
        T.op("act", lambda e: e.activation(out=siluc, in_=sm("c"), func=AF.Sigmoid), r=[SMB], w=[MODB])
        T.op("dve", lambda e: e.tensor_tensor(out=siluc, in0=siluc, in1=sm("c"), op=ALU.mult), r=[SMB, MODB], w=[MODB])
        for k in range(8):
            T.op("dve", lambda e, k=k: e.tensor_copy(out=sbc[:, k, :], in_=siluc[:, k:k + 1].to_broadcast([128, 128])),
                 r=[MODB], w=[SBCB])
        wada_v = wada_d.rearrange("(kc p) f -> p kc f", p=128)
        col_secs = [0, 1, 3, 4]
        order = [0, 1, 3, 4, 2, 5]
        SILB = Buf()
        MC = [Buf() for _ in range(4)]
        rowb = carve(176 * KB, [D])
        ROWB = Buf()
        one1 = carve(180 * KB, [4])
        T.op("dve", lambda e: e.memset(one1, 1.0), w=[CONST])

        def issue_sec(si):
            sec = order[si]
            slot = si % 3
            T.dma(wst[slot], wada_v[:, :, sec * D:(sec + 1) * D], w=[WSTB[slot]])

        def proc_sec(si):
            sec = order[si]
            slot = si % 3
            if sec in col_secs:
                ci = col_secs.index(sec)
                pb_i = 4 + si % 2
                pb = PB[pb_i]
                for half in range(2):
                    for k in range(8):
                        T.op("pe", lambda e, k=k: e.matmul(
                            psum[0:1, pb_i, :], lhsT=siluc[:, k:k + 1], rhs=wst[slot][:, k, half * 512:(half + 1) * 512],
                            start=(k == 0), stop=(k == 7)), r=[WSTB[slot], MODB], w=[pb], sig=(k == 7))
                    T.op("dve", lambda e: e.tensor_copy(out=rowb[0:1, half * 512:(half + 1) * 512], in_=psum[0:1, pb_i, :]),
                         r=[pb], w=[ROWB])
                for fc in range(8):
                    T.op("pe", lambda e, fc=fc: e.matmul(
                        psum[:, pb_i, fc:fc + 1], lhsT=rowb[0:1, fc * 128:(fc + 1) * 128], rhs=one1[0:1, 0:1],
                        start=True, stop=True), r=[ROWB, CONST], w=[pb], sig=(fc == 7))
                T.op("dve", lambda e: e.tensor_tensor(
                    out=modc[:, ci * 8:(ci + 1) * 8], in0=psum[:, pb_i, 0:8], in1=sm("bada", sec * 8, sec * 8 + 8), op=ALU.add),
                    r=[pb, SMB], w=[MC[ci]])
            else:
                GB = GB1 if sec == 2 else GB2
                for half in range(2):
                    pb_i = 6 + half
                    for k in range(8):
                        T.op("pe", lambda e, k=k: e.matmul(
                            psum[:, pb_i, :], lhsT=sbc[:, k, :], rhs=wst[slot][:, k, half * 512:(half + 1) * 512],
                            start=(k == 0), stop=(k == 7)), r=[WSTB[slot], SBCB], w=[PB[pb_i]], sig=(k == 7))
                    T.op("dve", lambda e: e.tensor_tensor(
                        out=GB[:, half * 512:(half + 1) * 512], in0=psum[:, pb_i, :], in1=GB[:, half * 512:(half + 1) * 512],
                        op=ALU.add), r=[PB[pb_i], SMB], w=[SMB])

        issue_sec(0)
        issue_sec(1)
        issue_sec(2)
        proc_sec(0)
        proc_sec(1)
        T.op("dve", lambda e: e.scalar_tensor_tensor(out=a1c, in0=modc[:, 8:16], scalar=1.0, in1=sm("g1"), op0=ALU.add, op1=ALU.mult),
             r=[MC[0], MC[1], SMB], w=[MODB])
        sh1c = modc[:, 0:8]
        sh2c = modc[:, 16:24]
        lamv = sm("lam").rearrange("p (a b) -> p a b", a=4)
        T.op("dve", lambda e: e.tensor_tensor(out=stat[:, 0:64], in0=lamv[:, 0, :], in1=lamv[:, 1, :], op=ALU.mult), r=[SMB], w=[MODB])
        T.op("dve", lambda e: e.tensor_reduce(out=lamt[:, 0:1], in_=stat[:, 0:64], axis=AX.X, op=ALU.add), r=[MODB], w=[MODB])
        T.op("dve", lambda e: e.tensor_tensor(out=stat[:, 0:64], in0=lamv[:, 2, :], in1=lamv[:, 3, :], op=ALU.mult), r=[SMB, MODB], w=[MODB])
        T.op("dve", lambda e: e.tensor_reduce(out=lamt[:, 1:2], in_=stat[:, 0:64], axis=AX.X, op=ALU.add), r=[MODB], w=[MODB])
        T.op("act", lambda e: e.activation(out=lamt[:, 2:4], in_=lamt[:, 0:2], func=AF.Exp), r=[MODB], w=[MODB])
        T.op("dve", lambda e: e.tensor_tensor(out=lamt[:, 4:5], in0=lamt[:, 3:4], in1=lamt[:, 2:3], op=ALU.subtract), r=[MODB], w=[MODB])
        T.op("dve", lambda e: e.tensor_scalar(out=lamt[:, 5:6], in0=lamt[:, 4:5], scalar1=-LAM_INIT, scalar2=None, op0=ALU.add), r=[MODB], w=[MODB])
        neglam = lamt[:, 5:6]

        def nt_stats(t, src_ap, src_bufs, tmp, TMPB):
            so = 4 * (t % 2)
            T.op("act", lambda e: e.activation(out=tmp, in_=src_ap, func=AF.Square, accum_out=stat[:, so:so + 1]),
                 r=src_bufs, w=[TMPB, MODB])
            T.op("act", lambda e: e.activation(out=stat[:, so + 1:so + 2], in_=stat[:, so:so + 1], func=AF.Ln, scale=1.0 / D, bias=EPS_AP),
                 r=[MODB, CONST], w=[MODB])
            T.op("act", lambda e: e.activation(out=stat[:, so + 2:so + 3], in_=stat[:, so + 1:so + 2], func=AF.Exp, scale=-0.5),
                 r=[MODB], w=[MODB])
            T.op("dve", lambda e: e.tensor_scalar(out=tmp, in0=src_ap, scalar1=stat[:, so + 2:so + 3], scalar2=None, op0=ALU.mult),
                 r=src_bufs + [MODB], w=[TMPB])

        def nt_trans(t, tmp, TMPB, ac, shc, dstT, DSTB, f32dst=None, F32B=None):
            for g in range(2):
                pb_i = (2 * t + g) % 4
                for j in range(4):
                    c = g * 4 + j
                    T.op("pe", lambda e, c=c, j=j, pb_i=pb_i: e.transpose(
                        out=psum[:, pb_i, j * 128:(j + 1) * 128], in_=tmp[:, c * 128:(c + 1) * 128], identity=ident_f),
                        r=[TMPB, SMB], w=[PB[pb_i]], sig=(j == 3))
                for j in range(4):
                    c = g * 4 + j
                    if f32dst is not None:
                        T.op("dve", lambda e, c=c, j=j, pb_i=pb_i: e.tensor_scalar(
                            out=f32dst[:, c, :], in0=psum[:, pb_i, j * 128:(j + 1) * 128], scalar1=ac[:, c:c + 1],
                            scalar2=shc[:, c:c + 1], op0=ALU.mult, op1=ALU.add), r=[PB[pb_i], MODB], w=[F32B])
                    elif g == 0:
                        T.op("dve", lambda e, c=c, j=j, pb_i=pb_i: e.tensor_scalar(
                            out=dstT[:, c, t * 128:(t + 1) * 128], in0=psum[:, pb_i, j * 128:(j + 1) * 128],
                            scalar1=ac[:, c:c + 1], scalar2=shc[:, c:c + 1], op0=ALU.mult, op1=ALU.add),
                            r=[PB[pb_i], MODB], w=[DSTB])
                    else:
                        T.op("act", lambda e, c=c, j=j, pb_i=pb_i: e.activation(
                            out=dstT[:, c, t * 128:(t + 1) * 128], in_=psum[:, pb_i, j * 128:(j + 1) * 128],
                            func=AF.Identity, scale=ac[:, c:c + 1], bias=shc[:, c:c + 1]),
                            r=[PB[pb_i], MODB], w=[DSTB])
            if f32dst is not None:
                T.op("pool", lambda e: e.tensor_copy(out=dstT[:, :, t * 128:(t + 1) * 128], in_=f32dst),
                     r=[F32B], w=[DSTB])

        epsc = RP.alloc([4])
        T.op("dve", lambda e: e.memset(epsc[:, 0:1], EPS), w=[CONST])
        T.op("dve", lambda e: e.memset(epsc[:, 1:2], 64.0 * EPS), w=[CONST])
        T.op("dve", lambda e: e.memset(epsc[:, 2:3], EPS / 0.64), w=[CONST])
        EPS_AP = epsc[:, 0:1]
        EPS64_AP = epsc[:, 1:2]
        EPSD_AP = epsc[:, 2:3]

        x_v = x_d.rearrange("(t p) f -> t p f", p=128)

        scope('B')
        RB = Region(156 * KB, ARENA)
        xt = [RB.alloc([D]) for _ in range(2)]
        XTB = [Buf(), Buf()]
        tmpn = [RB.alloc([D]) for _ in range(2)]
        TMPNB = [Buf(), Buf()]
        for it in range(NT + 1):
            if it in (0, 3, 6):
                issue_sec(3 + it // 3)
            if it in (2, 5, 8, 11):
                proc_sec(2 + (it - 2) // 3)
            if it < NT:
                sl = it % 2
                T.dma(xt[sl], x_v[it], w=[XTB[sl]])
                nt_stats(it, xt[sl], [XTB[sl]], tmpn[sl], TMPNB[sl])
            if it >= 1:
                t = it - 1
                nt_trans(t, tmpn[t % 2], TMPNB[t % 2], a1c, sh1c, hT, HTB)
        T.op("dve", lambda e: e.scalar_tensor_tensor(out=a2c, in0=modc[:, 24:32], scalar=1.0, in1=sm("g2"), op0=ALU.add, op1=ALU.mult),
             r=[MC[2], MC[3], SMB], w=[MODB])
        win_v = win_d.rearrange("(kc p) f -> p kc f", p=128)

        class CProj:
            def __init__(self, R, R2=None):
                R2 = R if R2 is None else R2
                w0_ = R.alloc([8, 512], BF16)
                self.stg = [R.alloc([512]) for _ in range(4)]
                self.wsec = [w0_, R2.alloc([8, 512], BF16)]
                self.WB = [Buf(), Buf()]
                self.SB = [Buf() for _ in range(4)]
                R = R2
                self.sq = [R.alloc([512]) for _ in range(3)]
                self.SQB = [Buf() for _ in range(3)]
                self.qn = [R.alloc([512], BF16) for _ in range(3)]
                self.QNB = [Buf() for _ in range(3)]
                self.nsec = 0
                self.npiece = 0

            def prefetch(self, col0):
                i = self.nsec % 2
                self.nsec += 1
                for k in range(8):
                    p = self.npiece % 4
                    self.npiece += 1
                    T.dma(self.stg[p], win_v[:, k, col0:col0 + 512], w=[self.SB[p]])
                    T.op("pool", lambda e, k=k, p=p: e.tensor_copy(out=self.wsec[i][:, k, :], in_=self.stg[p]),
                         r=[self.SB[p]], w=[self.WB[i]])
                return i

            def qk_fns(self, wi, gname, is_q, dstT, DSTB):
                wsec, WB = self.wsec[wi], self.WB[wi]
                gt = sm(gname)

                def mm(t, gi):
                    bank = gi % 3
                    for k in range(8):
                        T.op("pe", lambda e, k=k: e.matmul(psum[:, bank, :], lhsT=hT[:, k, t * 128:(t + 1) * 128], rhs=wsec[:, k, :],
                                                           start=(k == 0), stop=(k == 7)), r=[HTB, WB], w=[PB[bank]], sig=(k == 7))

                def chain(t, gi):
                    bank = gi % 3
                    s3 = gi % 3
                    sq, SQB, qn, QNB = self.sq[s3], self.SQB[s3], self.qn[s3], self.QNB[s3]
                    so = 8 + 16 * (gi % 2)
                    T.op("act", lambda e: e.activation(out=sq, in_=psum[:, bank, :], func=AF.Square), r=[PB[bank]], w=[SQB])
                    T.op("dve", lambda e: e.tensor_reduce(out=stat[:, so:so + 8], in_=sq.rearrange("p (a b) -> p a b", a=8),
                                                          axis=AX.X, op=ALU.add), r=[SQB], w=[MODB])
                    if is_q:
                        T.op("act", lambda e: e.activation(out=stat[:, so + 8:so + 16], in_=stat[:, so:so + 8], func=AF.Sqrt,
                                                           scale=1.0, bias=EPS64_AP), r=[MODB, CONST], w=[MODB])
                    else:
                        T.op("act", lambda e: e.activation(out=stat[:, so + 8:so + 16], in_=stat[:, so:so + 8], func=AF.Sqrt,
                                                           scale=1.0 / 64, bias=EPS_AP), r=[MODB, CONST], w=[MODB])
                    T.op("dve", lambda e: e.reciprocal(out=stat[:, so:so + 8], in_=stat[:, so + 8:so + 16]), r=[MODB], w=[MODB])
                    T.op("dve", lambda e: e.tensor_tensor(
                        out=sq.rearrange("p (a b) -> p a b", a=8), in0=psum[:, bank, :].rearrange("p (a b) -> p a b", a=8),
                        in1=stat[:, so:so + 8].unsqueeze(2).to_broadcast([128, 8, 64]), op=ALU.mult),
                        r=[PB[bank], MODB], w=[SQB])
                    T.op("pool", lambda e: e.tensor_tensor(
                        out=qn.rearrange("p (a b) -> p a b", a=8), in0=sq.rearrange("p (a b) -> p a b", a=8),
                        in1=gt.unsqueeze(1).to_broadcast([128, 8, 64]), op=ALU.mult), r=[SQB, SMB], w=[QNB])

                def trans(t, gi):
                    s3 = gi % 3
                    tb_i = 3 + (gi % 2)
                    tb = psum[:, tb_i, :].bitcast(BF16)
                    for j in range(4):
                        T.op("pe", lambda e, j=j: e.transpose(out=tb[:, j * 128:(j + 1) * 128], in_=self.qn[s3][:, j * 128:(j + 1) * 128],
                                                              identity=ident_bf), r=[self.QNB[s3], CONST], w=[PB[tb_i]], sig=(j == 3))
                    T.op("act", lambda e: e.activation(
                        out=dstT[:, :, t * 128:(t + 1) * 128], in_=tb[:, 0:512].rearrange("p (a b) -> p a b", a=4), func=AF.Copy),
                        r=[PB[tb_i]], w=[DSTB])

                return mm, chain, trans

            def run(self, fns_list, hook=None):
                items = [(s_, t) for s_ in range(len(fns_list)) for t in range(NT)]
                n = len(items)
                for it in range(n + 3):
                    if it == NT and hook is not None:
                        hook()
                    if it < n:
                        s_, t = items[it]
                        fns_list[s_][0](t, it)
                    if 1 <= it < n + 1:
                        s_, t = items[it - 1]
                        fns_list[s_][1](t, it - 1)
                    if it >= 3:
                        s_, t = items[it - 3]
                        fns_list[s_][2](t, it - 3)

        cpA = CProj(Region(190 * KB, ARENA), Region(172 * KB, 190 * KB))
        wA0 = cpA.prefetch(0)
        T.barrier()
        if debug:
            T.dma(dbg_hT, hT, r=[HTB])
            T.dma(dbg_sm, arena[:, 0:28 * KB // 4], r=[SMB, MODB, CONST])
            T.barrier()

        scope('CA')
        qTa = carve(76 * KB, [4, S], BF16)
        kTa = carve(156 * KB, [4, S], BF16)
        Va = [carve(ACC0 + p * 16 * KB, [NT, 512], BF16) for p in range(3)]
        QTAB, KTAB, VAB = Buf(), Buf(), Buf()
        cp = cpA
        w0 = wA0
        w1 = cp.prefetch(512)
        w2box = []
        cp.run([cp.qk_fns(w0, "gqa", True, qTa, QTAB), cp.qk_fns(w1, "gka", False, kTa, KTAB)],
               hook=lambda: w2box.append(cp.prefetch(1024)))
        w2 = w2box[0]
        wsec, WB = cp.wsec[w2], cp.WB[w2]

        def tok_cols(p, blk):
            if p == 0:
                return slice(blk * 128, (blk + 1) * 128)
            if p == 1:
                r4, n = blk // 4, blk % 4
                return slice(512 * n + r4, 512 * n + 512, 4)
            return slice(blk, S, 16)

        vi = 0
        for p in range(3):
            for blk in range(NT):
                pb_i = vi % 3
                cols = tok_cols(p, blk)
                for k in range(8):
                    T.op("pe", lambda e, k=k: e.matmul(psum[:, pb_i, :], lhsT=hT[:, k, cols], rhs=wsec[:, k, :],
                                                       start=(k == 0), stop=(k == 7)), r=[HTB, WB], w=[PB[pb_i]], sig=(k == 7))
                if vi % 2 == 0:
                    T.op("act", lambda e: e.activation(out=Va[p][:, blk, :], in_=psum[:, pb_i, :], func=AF.Copy), r=[PB[pb_i]], w=[VAB])
                else:
                    T.op("dve", lambda e: e.tensor_copy(out=Va[p][:, blk, :], in_=psum[:, pb_i, :]), r=[PB[pb_i]], w=[VAB])
                vi += 1
        T.barrier()
        if debug:
            T.dma(dbg_qk[:, 0], qTa, r=[QTAB])
            T.dma(dbg_qk[:, 1], kTa, r=[KTAB])
            T.barrier()

        scope('D')
        RD = Region(172 * KB, ARENA)
        biasr = [RD.alloc([5, 128]) for _ in range(2)]
        BIASB = [Buf(), Buf()]
        stmp = [RD.alloc([512]) for _ in range(2)]
        STB = [Buf(), Buf()]
        ptl = [RD.alloc([512], BF16) for _ in range(2)]
        PTB = [Buf(), Buf()]
        rtl = [RD.alloc([512]) for _ in range(2)]
        RTB = [Buf(), Buf()]
        OB = [PB[4], PB[5], PB[6], PB[7]]

        def head_units(h):
            units = []
            for p in range(3):
                nb = [16, 4, 1][p]
                ngrp = NT // nb
                for prev in (0, 1):
                    if p == 2 and prev:
                        continue
                    tiles = []
                    for grp in range(ngrp):
                        for n in range(nb):
                            if prev and n == 0:
                                continue
                            blk = grp * nb + n
                            qc = tok_cols(p, blk)
                            kc = tok_cols(p, blk - 1) if prev else qc
                            if p == 0:
                                outs = [(slice(0, 128), blk // 4, slice((blk % 4) * 128, (blk % 4) * 128 + 128))]
                            elif p == 1:
                                outs = [(slice(0, 128), n, slice(grp, 512, 4))]
                            else:
                                outs = [(slice(32 * g, 32 * g + 32), g, slice(blk, 512, 16)) for g in range(4)]
                            tiles.append((kc, qc, p, blk - 1 if prev else blk, outs))
                    bidx = [0, 2, 4][p] + prev
                    for i in range(0, len(tiles), 4):
                        units.append((bidx, tiles[i:i + 4]))
            return units

        allu = []
        for h in range(8):
            hu = head_units(h)
            for i, u in enumerate(hu):
                allu.append((h, i == 0, i == len(hu) - 1, u))
        started = {}

        def emit_bias(h):
            bs = biasr[h % 2]
            for bi, (src, dil) in enumerate([("dcur", 1), ("dprev", 1), ("dcur", 4), ("dprev", 4), ("dcur", 16)]):
                T.op("pool", lambda e, bi=bi, src=src, dil=dil: e.tensor_scalar(
                    out=bs[:, bi, :], in0=sm(src), scalar1=-SLOPES_A[h] * dil, scalar2=None, op0=ALU.mult),
                    r=[SMB], w=[BIASB[h % 2]])

        def emit_qk(g):
            h, first, last_u, (bidx, tiles) = allu[g]
            j, hh = h // 2, h % 2
            if first:
                emit_bias(h)
            sb_i = g % 4
            nt_ = len(tiles)
            for i, (kc, qc, p, vblk, outs) in enumerate(tiles):
                T.op("pe", lambda e, i=i, kc=kc, qc=qc: e.matmul(
                    psum[:, sb_i, i * 128:(i + 1) * 128], lhsT=kTa[64 * hh:64 * hh + 64, j, kc], rhs=qTa[64 * hh:64 * hh + 64, j, qc],
                    start=True, stop=True), r=[KTAB, QTAB], w=[PB[sb_i]], sig=(i == nt_ - 1))

        def emit_rest(g):
            h, first, last_u, (bidx, tiles) = allu[g]
            j, hh = h // 2, h % 2
            nlo, dlo = (0, 64) if hh == 0 else (64, 0)
            bs = biasr[h % 2]
            sb_i = g % 4
            s2 = g % 2
            nt_ = len(tiles)
            T.op("dve", lambda e: e.tensor_tensor(
                out=stmp[s2][:, 0:nt_ * 128].rearrange("p (a b) -> p a b", a=nt_),
                in0=psum[:, sb_i, 0:nt_ * 128].rearrange("p (a b) -> p a b", a=nt_),
                in1=bs[:, bidx, :].unsqueeze(1).to_broadcast([128, nt_, 128]), op=ALU.add),
                r=[PB[sb_i], BIASB[h % 2]], w=[STB[s2]])
            T.op("act", lambda e: e.activation(out=ptl[s2][:, 0:nt_ * 128], in_=stmp[s2][:, 0:nt_ * 128], func=AF.Exp),
                 r=[STB[s2]], w=[PTB[s2]])

        def emit_pv(g):
            h, first, last_u, (bidx, tiles) = allu[g]
            j, hh = h // 2, h % 2
            nlo, dlo = (0, 64) if hh == 0 else (64, 0)
            s2 = g % 2
            nt_ = len(tiles)
            if first:
                started.clear()
            for i, (kc, qc, p, vblk, outs) in enumerate(tiles):
                for oi, (msl, bank, ocols) in enumerate(outs):
                    last = (i == nt_ - 1 and oi == len(outs) - 1)
                    mv = ptl[s2][:, i * 128:(i + 1) * 128][:, msl]
                    st_n = (bank, 0) not in started
                    started[(bank, 0)] = 1
                    T.op("pe", lambda e, mv=mv, bank=bank, ocols=ocols, st_n=st_n, p=p, vblk=vblk: e.matmul(
                        psum[nlo:nlo + 64, 4 + bank, ocols], lhsT=Va[p][:, vblk, h * 64:(h + 1) * 64], rhs=mv,
                        start=st_n, stop=True, skip_group_check=True), r=[PTB[s2], VAB], w=[OB[bank]], sig=False)
                    st_d = (bank, 1) not in started
                    started[(bank, 1)] = 1
                    T.op("pe", lambda e, mv=mv, bank=bank, ocols=ocols, st_d=st_d: e.matmul(
                        psum[dlo:dlo + 64, 4 + bank, ocols], lhsT=ones_bf, rhs=mv,
                        start=st_d, stop=True, skip_group_check=True), r=[PTB[s2], CONST], w=[OB[bank]], sig=last)
            if last_u:
                for bank in range(4):
                    r2 = bank % 2
                    T.op("act", lambda e, bank=bank, r2=r2: e.activation(out=rtl[r2][dlo:dlo + 64, :], in_=psum[dlo:dlo + 64, 4 + bank, :],
                                                                       func=AF.Ln), r=[OB[bank]], w=[RTB[r2]])
                    T.op("act", lambda e, bank=bank, r2=r2: e.activation(out=rtl[r2][dlo:dlo + 64, :], in_=rtl[r2][dlo:dlo + 64, :],
                                                                       func=AF.Exp, scale=-1.0), r=[RTB[r2]], w=[RTB[r2]])
                    T.op("dve", lambda e, bank=bank, r2=r2: e.tensor_tensor(
                        out=catT[nlo:nlo + 64, j, bank * 512:(bank + 1) * 512], in0=psum[nlo:nlo + 64, 4 + bank, :],
                        in1=rtl[r2][dlo:dlo + 64, :], op=ALU.mult), r=[OB[bank], RTB[r2]], w=[CATB])

        emit_qk(0)
        emit_qk(1)
        emit_qk(2)
        emit_rest(0)
        for g in range(len(allu)):
            if g + 3 < len(allu):
                emit_qk(g + 3)
            if g + 1 < len(allu):
                emit_rest(g + 1)
            emit_pv(g)
        cpB = CProj(Region(187 * KB, ARENA), Region(156 * KB, 187 * KB))
        wB0 = cpB.prefetch(1536)
        T.barrier()

        scope('CB')
        qTb = carve(ACC0, [4, S], BF16)
        kTb = carve(ACC0 + 16 * KB, [4, S], BF16)
        Vb = carve(ACC0 + 32 * KB, [NT, 4 * 130], BF16)
        QTBB, KTBB, VBB = Buf(), Buf(), Buf()
        RC = Region(156 * KB, ARENA)
        cp = cpB
        w0 = wB0
        w1 = cp.prefetch(2048)
        w2box = []
        cp.run([cp.qk_fns(w0, "gqb", True, qTb, QTBB), cp.qk_fns(w1, "gkb", False, kTb, KTBB)],
               hook=lambda: w2box.append(cp.prefetch(2560)))
        w2 = w2box[0]
        wsec, WB = cp.wsec[w2], cp.WB[w2]
        Vb4 = Vb.rearrange("p t (h c) -> p t h c", h=4)
        T.op("pool", lambda e: e.memset(Vb, 1.0), w=[VBB])
        for t in range(NT):
            pb_i = t % 3
            for k in range(8):
                T.op("pe", lambda e, k=k: e.matmul(psum[:, pb_i, :], lhsT=hT[:, k, t * 128:(t + 1) * 128], rhs=wsec[:, k, :],
                                                   start=(k == 0), stop=(k == 7)), r=[HTB, WB], w=[PB[pb_i]], sig=(k == 7))
            T.op("act", lambda e: e.activation(out=Vb4[:, t, :, 0:128], in_=psum[:, pb_i, :].rearrange("p (h c) -> p h c", h=4),
                                               func=AF.Copy), r=[PB[pb_i]], w=[VBB])
        T.barrier()
        if debug:
            T.dma(dbg_qk[:, 2], qTb, r=[QTBB])
            T.dma(dbg_qk[:, 3], kTb, r=[KTBB])
            T.barrier()

        scope('E')
        RE = Region(156 * KB, ARENA)
        Pt_a = [RE.alloc([NT, 512], BF16) for _ in range(2)]
        Pt_b = [carve(HT0 + m_ * 16 * KB, [NT, 512], BF16) for m_ in range(2)]
        PtS = [Pt_a, Pt_b]
        PTBS = [[[Buf() for _ in range(NT)] for _ in range(2)] for _ in range(2)]
        Rw = [RE.alloc([4, 2, 130]) for _ in range(2)]
        RWB = [Buf(), Buf()]
        osb = [RE.alloc([128]) for _ in range(4)]
        OSB = [Buf() for _ in range(4)]
        onb2 = [RE.alloc([4, 128], BF16) for _ in range(2)]
        ONB2 = [Buf(), Buf()]
        junk = RE.alloc([128])
        JB = Buf()
        est = RE.alloc([32])
        ESB = Buf()
        bcol = sm("bcolb").rearrange("p (h d) -> p h d", h=4)
        gdiff = sm("gdiff")
        SCB = [0, 1, 6, 7]
        OUTB = [2, 3, 4]
        cnt = {"s": 0, "o": 0}
        chunks = [(h, Q) for h in range(4) for Q in range(4)]

        def emit_U(ci, m):
            h, Q = chunks[ci]
            Pt = PtS[ci % 2]
            PTB2 = PTBS[ci % 2]
            nkb = 4 * Q + 4
            for kb in range(nkb):
                q0 = max(512 * Q, 128 * kb)
                n = 512 * Q + 512 - q0
                lo = q0 - 512 * Q
                sb_i = SCB[cnt["s"] % 4]
                cnt["s"] += 1
                T.op("pe", lambda e: e.matmul(
                    psum[:, sb_i, 0:n], lhsT=kTb[64 * m:64 * m + 64, h, kb * 128:(kb + 1) * 128],
                    rhs=qTb[64 * m:64 * m + 64, h, q0:q0 + n], start=True, stop=True),
                    r=[KTBB, QTBB], w=[PB[sb_i]])
                d = kb - 4 * Q + 15
                T.op("act", lambda e: e.activation(
                    out=Pt[m][:, kb, lo:lo + n], in_=psum[:, sb_i, 0:n], func=AF.Exp, bias=bcol[:, h, d:d + 1]),
                    r=[PB[sb_i], SMB], w=[PTB2[m][kb]])
                if kb >= 4 * Q:
                    T.op("pool", lambda e: e.tensor_tensor(
                        out=Pt[m][:, kb, lo:lo + 128], in0=Pt[m][:, kb, lo:lo + 128], in1=tri_bf, op=ALU.mult),
                        r=[PTB2[m][kb], CONST], w=[PTB2[m][kb]])

        def emit_V(ci, m):
            h, Q = chunks[ci]
            Pt = PtS[ci % 2]
            PTB2 = PTBS[ci % 2]
            cp_ = ci % 2
            for qq in range(4):
                qb = 4 * Q + qq
                ob_i = OUTB[cnt["o"] % 3]
                cnt["o"] += 1
                for kb in range(qb + 1):
                    T.op("pe", lambda e, kb=kb: e.matmul(
                        psum[:, ob_i, 0:129], lhsT=Pt[m][:, kb, qq * 128:(qq + 1) * 128],
                        rhs=Vb4[:, kb, h, 0:129], start=(kb == 0), stop=(kb == qb)),
                        r=[PTB2[m][kb], VBB], w=[PB[ob_i]], sig=(kb == qb))
                T.op("dve", lambda e: e.tensor_copy(out=Rw[cp_][:, qq, m, 0:129], in_=psum[:, ob_i, 0:129]),
                     r=[PB[ob_i]], w=[RWB[cp_]])

        def emit_post(ci):
            h, Q = chunks[ci]
            cp_ = ci % 2
            R = Rw[cp_]
            RB = RWB[cp_]
            so = 16 * cp_
            T.op("dve", lambda e: e.reciprocal(out=est[:, so:so + 8].rearrange("p (q m) -> p q m", q=4), in_=R[:, :, :, 128]),
                 r=[RB], w=[ESB])
            for qq in range(4):
                o4 = qq
                T.op("dve", lambda e: e.tensor_tensor(out=est[:, so + 8 + qq:so + 9 + qq], in0=est[:, so + 2 * qq + 1:so + 2 * qq + 2],
                                                      in1=neglam, op=ALU.mult), r=[ESB, MODB], w=[ESB])
                T.op("dve", lambda e: e.tensor_scalar(out=osb[o4], in0=R[:, qq, 0, 0:128], scalar1=est[:, so + 2 * qq:so + 2 * qq + 1],
                                                      scalar2=None, op0=ALU.mult), r=[RB, ESB], w=[OSB[o4]])
                T.op("dve", lambda e: e.scalar_tensor_tensor(out=osb[o4], in0=R[:, qq, 1, 0:128], scalar=est[:, so + 8 + qq:so + 9 + qq],
                                                             in1=osb[o4], op0=ALU.mult, op1=ALU.add), r=[RB, ESB, OSB[o4]], w=[OSB[o4]])
                T.op("dve", lambda e: e.scalar_tensor_tensor(out=junk, in0=osb[o4], scalar=1.0, in1=osb[o4], op0=ALU.mult, op1=ALU.mult,
                                                             accum_out=est[:, so + 12 + qq:so + 13 + qq]), r=[OSB[o4]], w=[JB, ESB])
            T.op("act", lambda e: e.activation(out=est[:, so + 12:so + 16], in_=est[:, so + 12:so + 16], func=AF.Ln,
                                               scale=1.0 / (128 * 0.64), bias=EPSD_AP), r=[ESB, CONST], w=[ESB])
            T.op("act", lambda e: e.activation(out=est[:, so + 12:so + 16], in_=est[:, so + 12:so + 16], func=AF.Exp, scale=-0.5),
                 r=[ESB], w=[ESB])
            onb, ONB = onb2[cp_], ONB2[cp_]
            for qq in range(4):
                T.op("dve", lambda e: e.scalar_tensor_tensor(out=onb[:, qq, :], in0=osb[qq], scalar=est[:, so + 12 + qq:so + 13 + qq],
                                                             in1=gdiff, op0=ALU.mult, op1=ALU.mult), r=[OSB[qq], ESB, SMB], w=[ONB])

        def emit_post_b(ci):
            h, Q = chunks[ci]
            cp_ = ci % 2
            onb, ONB = onb2[cp_], ONB2[cp_]
            tb = psum[:, 5, :].bitcast(BF16)
            for qq in range(4):
                T.op("pe", lambda e: e.transpose(out=tb[:, qq * 128:(qq + 1) * 128], in_=onb[:, qq, :], identity=ident_bf),
                     r=[ONB, CONST], w=[PB[5]], sig=(qq == 3))
            T.op("act", lambda e: e.activation(out=catT[:, 4 + h, Q * 512:(Q + 1) * 512], in_=tb[:, 0:512], func=AF.Copy),
                 r=[PB[5]], w=[CATB])

        emit_U(0, 0)
        emit_U(0, 1)
        for ci in range(len(chunks)):
            nxt = ci + 1 < len(chunks)
            emit_V(ci, 0)
            if nxt:
                emit_U(ci + 1, 0)
            if ci >= 1:
                emit_post_b(ci - 1)
            emit_V(ci, 1)
            if nxt:
                emit_U(ci + 1, 1)
            emit_post(ci)
        emit_post_b(len(chunks) - 1)
        T.barrier()
        if debug:
            T.dma(dbg_catT, catT, r=[CATB])
            T.barrier()

        scope('F')
        RF = Region(156 * KB, ARENA)
        wo = RF.alloc([8, D], BF16)
        WOB = Buf()
        wos = [RF.alloc([2, D]) for _ in range(2)]
        WOSB = [Buf(), Buf()]
        xt = [RF.alloc([D]) for _ in range(2)]
        XTB = [Buf(), Buf()]
        wout_v = wout_d.rearrange("(kc p) f -> p kc f", p=128)
        for pc in range(4):
            sl = pc % 2
            T.dma(wos[sl], wout_v[:, 2 * pc:2 * pc + 2, :], w=[WOSB[sl]])
            T.op("pool", lambda e: e.tensor_tensor(out=wo[:, 2 * pc:2 * pc + 2, :], in0=wos[sl],
                                                   in1=GB1.unsqueeze(1).to_broadcast([128, 2, D]), op=ALU.mult),
                 r=[WOSB[sl], SMB], w=[WOB])
        ev_cast = (("c", "pool"), T.cnt["pool"])

        def f_tile(t):
            sl = t % 2
            T.dma(xt[sl], x_v[t], w=[XTB[sl]])
            for half in range(2):
                pb_i = 6 + half
                for k in range(8):
                    T.op("pe", lambda e, k=k: e.matmul(psum[:, pb_i, :], lhsT=catT[:, k, t * 128:(t + 1) * 128],
                                                       rhs=wo[:, k, half * 512:(half + 1) * 512], start=(k == 0), stop=(k == 7)),
                         r=[CATB, WOB], w=[PB[pb_i]], sig=(k == 7))
                T.op("dve", lambda e: e.tensor_tensor(out=acc[:, t, half * 512:(half + 1) * 512], in0=psum[:, pb_i, :],
                                                      in1=xt[sl][:, half * 512:(half + 1) * 512], op=ALU.add),
                     r=[PB[pb_i], XTB[sl]], w=[ACCB[t]])

        scope('G')
        RGa = Region(172 * KB, 188 * KB)
        RG = Region(196 * KB, ARENA)
        tmpn = [RGa.alloc([D]) for _ in range(2)]
        TMPNB = [Buf(), Buf()]
        h2f = [RGa.alloc([8, 128]) for _ in range(2)]
        H2FB = [Buf(), Buf()]
        for b_ in TMPNB + H2FB:
            b_.w = ev_cast
        lg = [RG.alloc([NE]) for _ in range(2)]
        LGB = [Buf(), Buf()]
        m8 = [RG.alloc([8]) for _ in range(2)]
        ex = [RG.alloc([NE]) for _ in range(2)]
        wr = sm("wr").rearrange("p (k e) -> p k e", k=8)
        GAB = Buf()
        GTB = Buf()

        Lg = RG.alloc([NT, NE])
        Wg = RG.alloc([NT, NE])
        Eq = RG.alloc([NT, NE])
        mt = RG.alloc([4, NT])
        LGALL = Buf()

        def g_router(t):
            s2 = t % 2
            for k in range(8):
                T.op("pe", lambda e, k=k: e.matmul(psum[:, 4 + s2, 0:NE], lhsT=h2f[s2][:, k, :], rhs=wr[:, k, :], start=(k == 0), stop=(k == 7)),
                     r=[H2FB[s2], SMB], w=[PB[4 + s2]], sig=(k == 7))
            T.op("dve", lambda e: e.tensor_tensor(out=Lg[:, t, :], in0=psum[:, 4 + s2, 0:NE], in1=sm("br"), op=ALU.add),
                 r=[PB[4 + s2], SMB], w=[LGALL])

        for it in range(NT + 3):
            if it < NT:
                f_tile(it)
            if 1 <= it < NT + 1:
                t = it - 1
                nt_stats(t, acc[:, t, :], [ACCB[t]], tmpn[t % 2], TMPNB[t % 2])
            if 2 <= it < NT + 2:
                t = it - 2
                nt_trans(t, tmpn[t % 2], TMPNB[t % 2], a2c, sh2c, hT, HTB, f32dst=h2f[t % 2], F32B=H2FB[t % 2])
            if it >= 3:
                g_router(it - 3)
        if debug:
            T.barrier()
            T.dma(dbg_x1, acc, r=ACCB)
            T.barrier()
        def bc3(ap2):
            return ap2.unsqueeze(2).to_broadcast([128, NT, NE])
        for r_ in range(4):
            src = Lg if r_ == 0 else Wg
            T.op("dve", lambda e: e.tensor_reduce(out=mt[:, r_, :], in_=src, axis=AX.X, op=ALU.max), r=[LGALL], w=[LGALL])
            if r_ < 3:
                T.op("dve", lambda e: e.tensor_tensor(out=Eq, in0=src, in1=bc3(mt[:, r_, :]), op=ALU.is_ge), r=[LGALL], w=[LGALL])
                T.op("dve", lambda e: e.scalar_tensor_tensor(out=Wg, in0=Eq, scalar=-1.0e9, in1=src, op0=ALU.mult, op1=ALU.add),
                     r=[LGALL], w=[LGALL])
        T.op("dve", lambda e: e.tensor_tensor(out=Eq, in0=Lg, in1=bc3(mt[:, 3, :]), op=ALU.is_ge), r=[LGALL], w=[LGALL])
        T.op("dve", lambda e: e.tensor_tensor(out=Wg, in0=Lg, in1=bc3(mt[:, 0, :]), op=ALU.subtract), r=[LGALL], w=[LGALL])
        T.op("act", lambda e: e.activation(out=Wg, in_=Wg, func=AF.Exp), r=[LGALL], w=[LGALL])
        T.op("dve", lambda e: e.tensor_tensor(out=Wg, in0=Wg, in1=Eq, op=ALU.mult), r=[LGALL], w=[LGALL])
        T.op("dve", lambda e: e.tensor_reduce(out=mt[:, 1, :], in_=Wg, axis=AX.X, op=ALU.add), r=[LGALL], w=[LGALL])
        T.op("dve", lambda e: e.reciprocal(out=mt[:, 2, :], in_=mt[:, 1, :]), r=[LGALL], w=[LGALL])
        T.op("dve", lambda e: e.tensor_tensor(out=G_all, in0=Wg, in1=bc3(mt[:, 2, :]), op=ALU.mult), r=[LGALL], w=[GAB])
        GTT = [Buf() for _ in range(NT)]

        def emit_gt(t):
            s2 = t % 2
            T.op("pe", lambda e: e.transpose(out=psum[0:NE, 6 + s2, 0:128], in_=G_all[:, t, :], identity=ident_f), r=[GAB, SMB], w=[PB[6 + s2]])
            T.op("act", lambda e: e.activation(out=GT[0:NE, t * 128:(t + 1) * 128], in_=psum[0:NE, 6 + s2, 0:128], func=AF.Copy),
                 r=[PB[6 + s2]], w=[GTT[t]])
        T.barrier()
        if debug:
            T.dma(dbg_h2T, hT, r=[HTB])
            T.barrier()

        scope('H')
        RH = Region(R30, ARENA)
        actT = RH.alloc([8, S], BF16)
        ACTB = [[Buf() for _ in range(4)] for _ in range(8)]
        wgu = [RH.alloc([8, 256], BF16) for _ in range(2)]
        WGUB = [Buf() for _ in range(2)]
        wdn = RH.alloc([8, D], BF16)
        WDB = [Buf() for _ in range(8)]
        stgd = RH.alloc([D])
        STGDB = Buf()
        stg = [RH.alloc([2 * D]) for _ in range(2)]
        STGB = [Buf(), Buf()]
        STGU = [Buf(), Buf()]
        gcb = [RH.alloc([512], BF16) for _ in range(2)]
        sgb = [RH.alloc([512], BF16) for _ in range(2)]
        aab = [RH.alloc([512], BF16) for _ in range(2)]
        GCB, SGB, AAB = [Buf(), Buf()], [Buf(), Buf()], [Buf(), Buf()]
        bgu = sm("bgu").rearrange("p (e c) -> p e c", e=NE)
        GB2K = GB1
        GKB = SMB
        T.op("dve", lambda e: e.tensor_scalar(out=GB2K, in0=GB2, scalar1=1.0 / 1.702, scalar2=None, op0=ALU.mult), r=[SMB], w=[GKB])
        bds = stg[0][0:NE, 0:D]
        T.dma(bds, bd_d, w=[STGB[0]])
        T.op("pool", lambda e: e.tensor_tensor(out=bdp[0:NE, :], in0=bds, in1=GB2[0:NE, :], op=ALU.mult), r=[STGB[0], SMB], w=[GTB])
        def emit_bias_term(t0, t1):
            for t in range(t0, t1):
                for half in range(2):
                    pb_i = 4 + (2 * t + half) % 4
                    T.op("pe", lambda e: e.matmul(psum[:, pb_i, :], lhsT=GT[0:NE, t * 128:(t + 1) * 128], rhs=bdp[0:NE, half * 512:(half + 1) * 512],
                                                  start=True, stop=True), r=[GTB, GTT[t]], w=[PB[pb_i]])
                    T.op("dve", lambda e: e.tensor_tensor(out=acc[:, t, half * 512:(half + 1) * 512], in0=psum[:, pb_i, :],
                                                          in1=acc[:, t, half * 512:(half + 1) * 512], op=ALU.add),
                         r=[PB[pb_i], ACCB[t]], w=[ACCB[t]])
        def gu_prefetch(q):
            ex_q, ffp_q = q // 8, q % 8
            wgu_q = wgu_d[ex_q].rearrange("(kc p) f -> p kc f", p=128)
            ss = q % 2
            sv = stg[ss].rearrange("p (k u f) -> p k u f", k=8, u=2)
            T.dma(sv[:, :, 0, :], wgu_q[:, :, ffp_q * 128:(ffp_q + 1) * 128], w=[STGB[ss]])
            T.dma(sv[:, :, 1, :], wgu_q[:, :, D + ffp_q * 128:D + (ffp_q + 1) * 128], w=[STGU[ss]])
            T.op("act", lambda e: e.activation(out=wgu[q % 2], in_=stg[ss].rearrange("p (k c) -> p k c", k=8), func=AF.Copy),
                 r=[STGB[ss], STGU[ss]], w=[WGUB[q % 2]])

        ui = 0
        gu_prefetch(0)
        for ex_i in range(NE):
            wd_e = wd_d[ex_i].rearrange("(kc p) f -> p kc f", p=128)
            for ffp in range(8):
                q = ex_i * 8 + ffp
                if q + 1 < NE * 8:
                    gu_prefetch(q + 1)
                T.dma(stgd, wd_e[:, ffp, :], w=[STGDB])
                gs = q % 2
                for tc in range(4):
                    u2 = ui % 2
                    ui += 1
                    pg, pu = 2 * u2, 2 * u2 + 1
                    for k in range(8):
                        T.op("pe", lambda e, k=k: e.matmul(psum[:, pg, :], lhsT=wgu[gs][:, k, 0:128], rhs=hT[:, k, tc * 512:(tc + 1) * 512],
                                                           start=(k == 0), stop=(k == 7)), r=[WGUB[gs], HTB], w=[PB[pg]], sig=(k == 7))
                    for k in range(8):
                        T.op("pe", lambda e, k=k: e.matmul(psum[:, pu, :], lhsT=wgu[gs][:, k, 128:256], rhs=hT[:, k, tc * 512:(tc + 1) * 512],
                                                           start=(k == 0), stop=(k == 7)), r=[WGUB[gs], HTB], w=[PB[pu]], sig=(k == 7))
                    T.op("dve", lambda e: e.tensor_scalar(out=gcb[u2], in0=psum[:, pg, :], scalar1=bgu[:, ex_i, ffp:ffp + 1], scalar2=7.0,
                                                          op0=ALU.add, op1=ALU.min), r=[PB[pg], SMB], w=[GCB[u2]])
                    T.op("act", lambda e: e.activation(out=sgb[u2], in_=gcb[u2], func=AF.Silu, scale=1.702), r=[GCB[u2]], w=[SGB[u2]])
                    T.op("dve", lambda e: e.tensor_scalar(out=aab[u2], in0=psum[:, pu, :], scalar1=bgu[:, ex_i, 8 + ffp:9 + ffp], scalar2=-7.0,
                                                          op0=ALU.add, op1=ALU.max), r=[PB[pu], SMB], w=[AAB[u2]])
                    T.op("dve", lambda e: e.tensor_scalar(out=aab[u2], in0=aab[u2], scalar1=7.0, scalar2=1.0, op0=ALU.min, op1=ALU.add),
                         r=[AAB[u2]], w=[AAB[u2]])
                    T.op("pool", lambda e: e.tensor_tensor(out=actT[:, ffp, tc * 512:(tc + 1) * 512], in0=sgb[u2], in1=aab[u2], op=ALU.mult),
                         r=[SGB[u2], AAB[u2]], w=[ACTB[ffp][tc]])
                T.op("dve", lambda e: e.tensor_tensor(out=wdn[:, ffp, :], in0=stgd, in1=GB2K, op=ALU.mult),
                     r=[STGDB, GKB], w=[WDB[ffp]])
                if ex_i == 0:
                    emit_gt(2 * ffp)
                    emit_gt(2 * ffp + 1)
                    emit_bias_term(2 * ffp, 2 * ffp + 2)
            for t in range(NT):
                for half in range(2):
                    pb_i = 4 + (2 * t + half) % 4
                    for f in range(8):
                        T.op("pe", lambda e, f=f: e.matmul(psum[:, pb_i, :], lhsT=actT[:, f, t * 128:(t + 1) * 128],
                                                           rhs=wdn[:, f, half * 512:(half + 1) * 512], start=(f == 0), stop=(f == 7)),
                             r=[ACTB[f][t // 4], WDB[f]], w=[PB[pb_i]], sig=(f == 7))
                    T.op("dve", lambda e: e.scalar_tensor_tensor(
                        out=acc[:, t, half * 512:(half + 1) * 512], in0=psum[:, pb_i, :], scalar=G_all[:, t, ex_i:ex_i + 1],
                        in1=acc[:, t, half * 512:(half + 1) * 512], op0=ALU.mult, op1=ALU.add),
                        r=[PB[pb_i], GAB, ACCB[t]], w=[ACCB[t]])
        scope('OUT')
        out_v = out_d.rearrange("(t p) f -> t p f", p=128)
        for t in range(NT):
            T.dma(out_v[t], acc[:, t, :], r=[ACCB[t]])
        T.barrier()
        scope(None)
    return nc


def _prep_smalls(inp, b):
    sm = np.zeros((128, NS), np.float32)

    def put(name, arr):
        o, w = _SM[name]
        arr = np.asarray(arr, np.float32)
        assert arr.shape == (128, w), (name, arr.shape)
        sm[:, o:o + w] = arr

    def cols(v, n):
        return np.asarray(v, np.float32).reshape(n, 128).T

    def bc(v):
        v = np.asarray(v, np.float32).reshape(1, -1)
        return np.broadcast_to(v, (128, v.shape[1]))

    put("c", cols(inp["c"][b], 8))
    put("bada", cols(inp["b_ada"][0], 48))
    put("g1", cols(inp["norm1_g"][0], 8))
    put("g2", cols(inp["norm2_g"][0], 8))
    put("gqa", bc(inp["a_q_norm_g"][0]))
    put("gka", bc(inp["a_k_norm_g"][0]))
    put("gqb", bc(inp["b_q_norm_g"][0]))
    put("gkb", bc(inp["b_k_norm_g"][0]))
    put("lam", bc(np.concatenate([inp["lambda_q1"][0], inp["lambda_k1"][0], inp["lambda_q2"][0], inp["lambda_k2"][0]])))
    put("gdiff", bc(inp["diff_norm_g"][0]))
    put("br", bc(inp["b_router"][0]))
    put("bgu", np.asarray(inp["b_gate_up"][0], np.float32).reshape(NE, 16, 128).transpose(2, 0, 1).reshape(128, NE * 16))
    put("wr", np.asarray(inp["w_router"][0], np.float32).reshape(8, 128, NE).transpose(1, 0, 2).reshape(128, 8 * NE))
    put("bg1", bc(inp["b_ada"][0][2 * D:3 * D]))
    put("bg2", bc(inp["b_ada"][0][5 * D:6 * D]))
    jj = np.arange(128)[:, None].astype(np.float64)
    ii = np.arange(128)[None, :].astype(np.float64)
    put("ident", np.eye(128))
    put("dcur", np.where(ii >= jj, ii - jj, BIG))
    put("dprev", np.where(jj >= ii, 128 + ii - jj, BIG))
    put("tri", (jj <= ii).astype(np.float32))
    bcol = np.zeros((128, 4, 19))
    for h in range(4):
        for d in range(19):
            bcol[:, h, d] = SLOPES_B[h] * (128.0 * (d - 15) + np.arange(128) - 256.0)
    put("bcolb", bcol.reshape(128, 76))
    return sm


_NC_CACHE = {}


def kernel(**inputs):
    inp = {k: np.asarray(v) for k, v in inputs.items()}
    if "nc" not in _NC_CACHE:
        _NC_CACHE["nc"] = build_nc()
    nc = _NC_CACHE["nc"]
    shared = {
        "w_ada": np.ascontiguousarray(inp["w_ada"][0], np.float32),
        "w_in": np.ascontiguousarray(inp["w_in"][0], np.float32),
        "w_out": np.ascontiguousarray(inp["w_out"][0], np.float32),
        "w_gate_up": np.ascontiguousarray(inp["w_gate_up"][0], np.float32),
        "w_down": np.ascontiguousarray(inp["w_down"][0], np.float32),
        "b_down": np.ascontiguousarray(inp["b_down"][0], np.float32),
    }
    in_maps = []
    for b in range(8):
        m = dict(shared)
        m["x"] = np.ascontiguousarray(inp["x"][b], np.float32)
        m["smalls"] = _prep_smalls(inp, b)
        in_maps.append(m)
    res = run_bass_kernel_spmd(nc, in_maps, core_ids=list(range(8)))
    return np.stack([np.asarray(r["out"], np.float32) for r in res.results], axis=0)
```

```python
import numpy as np
from contextlib import ExitStack
import concourse.bass as bass
import concourse.mybir as mybir
from concourse.bass_utils import run_bass_kernel_spmd

F32 = mybir.dt.float32
BF16 = mybir.dt.bfloat16
ALU = mybir.AluOpType
AF = mybir.ActivationFunctionType
AX = mybir.AxisListType

S = 2048
D = 1024
NT = 16
NE = 32
EPS = 1e-6
KB = 1024
ARENA = 206 * KB
SLOPES_A = [2.0 ** (-8.0 * (i + 1) / 8) for i in range(8)]
SLOPES_B = [2.0 ** (-8.0 * (i + 1) / 4) for i in range(4)]
LAM_INIT = 0.8 - 0.6 * 1.0
BIG = 1.0e6

_SM = {}
_off = 0
for _n, _w in [("c", 8), ("bada", 48), ("g1", 8), ("g2", 8), ("gqa", 64), ("gka", 64), ("gqb", 64),
               ("gkb", 64), ("lam", 256), ("gdiff", 128), ("br", 32), ("bcolb", 76), ("bgu", 512),
               ("wr", 256), ("ident", 128), ("dcur", 128), ("dprev", 128), ("tri", 128),
               ("bg1", 1024), ("bg2", 1024)]:
    _SM[_n] = (_off, _w)
    _off += _w
NS = _off


class Buf:
    __slots__ = ("w", "r")

    def __init__(self):
        self.w = None
        self.r = {}


class Tracker:
    NDS = 24

    def __init__(self, nc, es):
        self.nc = nc
        self.eng = dict(pe=nc.tensor, act=nc.scalar, dve=nc.vector, pool=nc.gpsimd, sp=nc.sync)
        self.sem = {k: es.enter_context(nc.semaphore("s_" + k)) for k in ("pe", "act", "dve", "pool")}
        self.cnt = {k: 0 for k in self.sem}
        self.seen = {k: {} for k in self.eng}
        self.pending = {k: [] for k in self.eng}
        self.dsems = [es.enter_context(nc.semaphore("d%d" % i)) for i in range(self.NDS)]
        self.dcnt = [0] * self.NDS
        self.dnext = 0

    def wait(self, e, key, val):
        if self.seen[e].get(key, 0) >= val:
            return
        if key == ("c", "pe") and e == "pe":
            return
        sem = self.sem[key[1]] if key[0] == "c" else self.dsems[key[1]]
        self.eng[e].wait_ge(sem, val)
        self.seen[e][key] = val

    def _deps(self, e, r, w):
        for b in r:
            if b.w is not None:
                self.wait(e, *b.w)
        for b in w:
            if b.w is not None:
                self.wait(e, *b.w)
            for k, v in b.r.items():
                self.wait(e, k, v)

    def op(self, e, fn, r=(), w=(), sig=True):
        self._deps(e, r, w)
        ins = fn(self.eng[e])
        self.pending[e].append((r, w))
        if sig:
            self.cnt[e] += 1
            ins.then_inc(self.sem[e], 1)
            key = ("c", e)
            val = self.cnt[e]
            for (rr, ww) in self.pending[e]:
                for b in rr:
                    b.r[key] = val
                for b in ww:
                    b.w = (key, val)
                    b.r = {}
            self.pending[e] = []
        return ins

    def dma(self, out, in_, r=(), w=(), q="sp"):
        self._deps(q, r, w)
        i = self.dnext
        self.dnext = (i + 1) % self.NDS
        key = ("d", i)
        if self.dcnt[i] > 0:
            self.wait(q, key, self.dcnt[i])
        self.dcnt[i] += 16
        self.eng[q].dma_start(out=out, in_=in_).then_inc(self.dsems[i], 16)
        val = self.dcnt[i]
        for b in r:
            b.r[key] = val
        for b in w:
            b.w = (key, val)
            b.r = {}

    def barrier(self):
        for e in ("pe", "act", "dve", "pool", "sp"):
            for k in ("pe", "act", "dve", "pool"):
                if self.cnt[k] > 0 and k != e:
                    self.wait(e, ("c", k), self.cnt[k])
            for i in range(self.NDS):
                if self.dcnt[i] > 0:
                    self.wait(e, ("d", i), self.dcnt[i])


def _dsize(dt):
    return 2 if dt == BF16 else 4


def build_nc(debug=False):
    nc = bass.Bass("TRN2", target_bir_lowering=False)
    x_d = nc.dram_tensor("x", [S, D], F32, kind="ExternalInput").ap()
    sm_d = nc.dram_tensor("smalls", [128, NS], F32, kind="ExternalInput").ap()
    wada_d = nc.dram_tensor("w_ada", [D, 6 * D], F32, kind="ExternalInput").ap()
    win_d = nc.dram_tensor("w_in", [D, 3 * D], F32, kind="ExternalInput").ap()
    wout_d = nc.dram_tensor("w_out", [D, D], F32, kind="ExternalInput").ap()
    wgu_d = nc.dram_tensor("w_gate_up", [NE, D, 2 * D], F32, kind="ExternalInput").ap()
    wd_d = nc.dram_tensor("w_down", [NE, D, D], F32, kind="ExternalInput").ap()
    bd_d = nc.dram_tensor("b_down", [NE, D], F32, kind="ExternalInput").ap()
    out_d = nc.dram_tensor("out", [S, D], F32, kind="ExternalOutput").ap()
    if debug:
        dbg_hT = nc.dram_tensor("dbg_hT", [128, 8, S], BF16, kind="ExternalOutput").ap()
        dbg_catT = nc.dram_tensor("dbg_catT", [128, 8, S], BF16, kind="ExternalOutput").ap()
        dbg_h2T = nc.dram_tensor("dbg_h2T", [128, 8, S], BF16, kind="ExternalOutput").ap()
        dbg_x1 = nc.dram_tensor("dbg_x1", [128, NT, D], F32, kind="ExternalOutput").ap()
        dbg_sm = nc.dram_tensor("dbg_sm", [128, 28 * KB // 4], F32, kind="ExternalOutput").ap()
        dbg_qk = nc.dram_tensor("dbg_qk", [128, 4, 4, S], BF16, kind="ExternalOutput").ap()

    with ExitStack() as es:
        arena = es.enter_context(nc.sbuf_tensor("arena", [128, ARENA // 4], F32))
        psum = es.enter_context(nc.psum_tensor("psum", [128, 8, 512], F32))
        T = Tracker(nc, es)
        _sc = [None]

        def scope(name):
            if _sc[0] is not None:
                nc.leave_named_scope(_sc[0][0], _sc[0][1], False)
            _sc[0] = None
            if name is not None:
                sid, _ = nc.enter_named_scope(name, False)
                _sc[0] = (name, sid)
        PB = [Buf() for _ in range(8)]

        def carve(off, shape, dt=F32, parts=128):
            n = 1
            for s_ in shape:
                n *= s_
            nb = n * _dsize(dt)
            assert off % 4 == 0 and nb % 4 == 0 and off + nb <= ARENA, (off, nb)
            a = arena[0:parts, off // 4:(off + nb) // 4]
            if dt != F32:
                a = a.bitcast(dt)
            if len(shape) == 2:
                a = a.rearrange("p (a b) -> p a b", a=shape[0])
            elif len(shape) == 3:
                a = a.rearrange("p (a b c) -> p a b c", a=shape[0], b=shape[1])
            return a

        class Region:
            def __init__(self, start, end):
                self.p = start
                self.end = end

            def alloc(self, shape, dt=F32, parts=128):
                n = 1
                for s_ in shape:
                    n *= s_
                nb = (n * _dsize(dt) + 3) // 4 * 4
                off = self.p
                self.p += nb
                assert self.p <= self.end, ("region overflow", self.p, self.end)
                return carve(off, shape, dt, parts)

        RP = Region(0, 28 * KB)
        smalls = RP.alloc([NS])
        SMB = Buf()

        def sm(name, lo=0, hi=None):
            o, w_ = _SM[name]
            hi = w_ if hi is None else hi
            return smalls[:, o + lo:o + hi]

        ident_bf = RP.alloc([128], BF16)
        tri_bf = RP.alloc([128], BF16)
        ones_bf = RP.alloc([64], BF16)
        siluc = RP.alloc([8])
        modc = RP.alloc([32])
        a1c = RP.alloc([8])
        a2c = RP.alloc([8])
        G_all = RP.alloc([NT, NE])
        GT = RP.alloc([S], BF16)
        bdp = RP.alloc([D], BF16)
        lamt = RP.alloc([8])
        stat = RP.alloc([64])
        CONST = Buf()
        MODB = Buf()
        GB1 = sm("bg1")
        GB2 = sm("bg2")
        ident_f = sm("ident")

        ACC0, HT0, R30 = 28 * KB, 92 * KB, 124 * KB
        acc = carve(ACC0, [NT, D])
        ACCB = [Buf() for _ in range(NT)]
        hT = carve(HT0, [8, S], BF16)
        HTB = Buf()
        catT = carve(R30, [8, S], BF16)
        CATB = Buf()

        T.dma(smalls, sm_d, w=[SMB])
        T.op("dve", lambda e: e.tensor_copy(out=ident_bf, in_=ident_f), r=[SMB], w=[CONST])
        T.op("dve", lambda e: e.tensor_copy(out=tri_bf, in_=sm("tri")), r=[SMB], w=[CONST])
        T.op("dve", lambda e: e.memset(ones_bf, 1.0), w=[CONST])

        scope('A')
        wst = [carve(ACC0, [8, D]), carve(ACC0 + 32 * KB, [8, D]), carve(R30, [8, D])]
        WSTB = [Buf(), Buf(), Buf()]
        sbc = carve(172 * KB, [8, 128])
        SBCB = Buf()
        T.op("act", lambda e: e.activation(out=siluc, in_=sm("c"), func=AF.Sigmoid), r=[SMB], w=[MODB])
        T.op("dve", lambda e: e.tensor_tensor(out=siluc, in0=siluc, in1=sm("c"), op=ALU.mult), r=[SMB, MODB], w=[MODB])
        for k in range(8):
            T.op("dve", lambda e, k=k: e.tensor_copy(out=sbc[:, k, :], in_=siluc[:, k:k + 1].to_broadcast([128, 128])),
                 r=[MODB], w=[SBCB])
        wada_v = wada_d.rearrange("(kc p) f -> p kc f", p=128)
        col_secs = [0, 1, 3, 4]
        order = [0, 1, 3, 4, 2, 5]
        SILB = Buf()
        MC = [Buf() for _ in range(4)]
        rowb = carve(176 * KB, [D])
        ROWB = Buf()
        one1 = carve(180 * KB, [4])
        T.op("dve", lambda e: e.memset(one1, 1.0), w=[CONST])

        def issue_sec(si):
            sec = order[si]
            slot = si % 3
            T.dma(wst[slot], wada_v[:, :, sec * D:(sec + 1) * D], w=[WSTB[slot]])

        def proc_sec(si):
            sec = order[si]
            slot = si % 3
            if sec in col_secs:
                ci = col_secs.index(sec)
                pb_i = 4 + si % 2
                pb = PB[pb_i]
                for half in range(2):
                    for k in range(8):
                        T.op("pe", lambda e, k=k: e.matmul(
                            psum[0:1, pb_i, :], lhsT=siluc[:, k:k + 1], rhs=wst[slot][:, k, half * 512:(half + 1) * 512],
                            start=(k == 0), stop=(k == 7)), r=[WSTB[slot], MODB], w=[pb], sig=(k == 7))
                    T.op("dve", lambda e: e.tensor_copy(out=rowb[0:1, half * 512:(half + 1) * 512], in_=psum[0:1, pb_i, :]),
                         r=[pb], w=[ROWB])
                for fc in range(8):
                    T.op("pe", lambda e, fc=fc: e.matmul(
                        psum[:, pb_i, fc:fc + 1], lhsT=rowb[0:1, fc * 128:(fc + 1) * 128], rhs=one1[0:1, 0:1],
                        start=True, stop=True), r=[ROWB, CONST], w=[pb], sig=(fc == 7))
                T.op("dve", lambda e: e.tensor_tensor(
                    out=modc[:, ci * 8:(ci + 1) * 8], in0=psum[:, pb_i, 0:8], in1=sm("bada", sec * 8, sec * 8 + 8), op=ALU.add),
                    r=[pb, SMB], w=[MC[ci]])
            else:
                GB = GB1 if sec == 2 else GB2
                for half in range(2):
                    pb_i = 6 + half
                    for k in range(8):
                        T.op("pe", lambda e, k=k: e.matmul(
                            psum[:, pb_i, :], lhsT=sbc[:, k, :], rhs=wst[slot][:, k, half * 512:(half + 1) * 512],
                            start=(k == 0), stop=(k == 7)), r=[WSTB[slot], SBCB], w=[PB[pb_i]], sig=(k == 7))
                    T.op("dve", lambda e: e.tensor_tensor(
                        out=GB[:, half * 512:(half + 1) * 512], in0=psum[:, pb_i, :], in1=GB[:, half * 512:(half + 1) * 512],
                        op=ALU.add), r=[PB[pb_i], SMB], w=[SMB])

        issue_sec(0)
        issue_sec(1)
        issue_sec(2)
        proc_sec(0)
        proc_sec(1)
        T.op("dve", lambda e: e.scalar_tensor_tensor(out=a1c, in0=modc[:, 8:16], scalar=1.0, in1=sm("g1"), op0=ALU.add, op1=ALU.mult),
             r=[MC[0], MC[1], SMB], w=[MODB])
        sh1c = modc[:, 0:8]
        sh2c = modc[:, 16:24]
        lamv = sm("lam").rearrange("p (a b) -> p a b", a=4)
        T.op("dve", lambda e: e.tensor_tensor(out=stat[:, 0:64], in0=lamv[:, 0, :], in1=lamv[:, 1, :], op=ALU.mult), r=[SMB], w=[MODB])
        T.op("dve", lambda e: e.tensor_reduce(out=lamt[:, 0:1], in_=stat[:, 0:64], axis=AX.X, op=ALU.add), r=[MODB], w=[MODB])
        T.op("dve", lambda e: e.tensor_tensor(out=stat[:, 0:64], in0=lamv[:, 2, :], in1=lamv[:, 3, :], op=ALU.mult), r=[SMB, MODB], w=[MODB])
        T.op("dve", lambda e: e.tensor_reduce(out=lamt[:, 1:2], in_=stat[:, 0:64], axis=AX.X, op=ALU.add), r=[MODB], w=[MODB])
        T.op("act", lambda e: e.activation(out=lamt[:, 2:4], in_=lamt[:, 0:2], func=AF.Exp), r=[MODB], w=[MODB])
        T.op("dve", lambda e: e.tensor_tensor(out=lamt[:, 4:5], in0=lamt[:, 3:4], in1=lamt[:, 2:3], op=ALU.subtract), r=[MODB], w=[MODB])
        T.op("dve", lambda e: e.tensor_scalar(out=lamt[:, 5:6], in0=lamt[:, 4:5], scalar1=-LAM_INIT, scalar2=None, op0=ALU.add), r=[MODB], w=[MODB])
        neglam = lamt[:, 5:6]

        def nt_stats(t, src_ap, src_bufs, tmp, TMPB):
            so = 4 * (t % 2)
            T.op("act", lambda e: e.activation(out=tmp, in_=src_ap, func=AF.Square, accum_out=stat[:, so:so + 1]),
                 r=src_bufs, w=[TMPB, MODB])
            T.op("act", lambda e: e.activation(out=stat[:, so + 1:so + 2], in_=stat[:, so:so + 1], func=AF.Ln, scale=1.0 / D, bias=EPS_AP),
                 r=[MODB, CONST], w=[MODB])
            T.op("act", lambda e: e.activation(out=stat[:, so + 2:so + 3], in_=stat[:, so + 1:so + 2], func=AF.Exp, scale=-0.5),
                 r=[MODB], w=[MODB])
            T.op("dve", lambda e: e.tensor_scalar(out=tmp, in0=src_ap, scalar1=stat[:, so + 2:so + 3], scalar2=None, op0=ALU.mult),
                 r=src_bufs + [MODB], w=[TMPB])

        def nt_trans(t, tmp, TMPB, ac, shc, dstT, DSTB, f32dst=None, F32B=None):
            for g in range(2):
                pb_i = (2 * t + g) % 4
                for j in range(4):
                    c = g * 4 + j
                    T.op("pe", lambda e, c=c, j=j, pb_i=pb_i: e.transpose(
                        out=psum[:, pb_i, j * 128:(j + 1) * 128], in_=tmp[:, c * 128:(c + 1) * 128], identity=ident_f),
                        r=[TMPB, SMB], w=[PB[pb_i]], sig=(j == 3))
                for j in range(4):
                    c = g * 4 + j
                    if f32dst is not None:
                        T.op("dve", lambda e, c=c, j=j, pb_i=pb_i: e.tensor_scalar(
                            out=f32dst[:, c, :], in0=psum[:, pb_i, j * 128:(j + 1) * 128], scalar1=ac[:, c:c + 1],
                            scalar2=shc[:, c:c + 1], op0=ALU.mult, op1=ALU.add), r=[PB[pb_i], MODB], w=[F32B])
                    elif g == 0:
                        T.op("dve", lambda e, c=c, j=j, pb_i=pb_i: e.tensor_scalar(
                            out=dstT[:, c, t * 128:(t + 1) * 128], in0=psum[:, pb_i, j * 128:(j + 1) * 128],
                            scalar1=ac[:, c:c + 1], scalar2=shc[:, c:c + 1], op0=ALU.mult, op1=ALU.add),
                            r=[PB[pb_i], MODB], w=[DSTB])
                    else:
                        T.op("act", lambda e, c=c, j=j, pb_i=pb_i: e.activation(
                            out=dstT[:, c, t * 128:(t + 1) * 128], in_=psum[:, pb_i, j * 128:(j + 1) * 128],
                            func=AF.Identity, scale=ac[:, c:c + 1], bias=shc[:, c:c + 1]),
                            r=[PB[pb_i], MODB], w=[DSTB])
            if f32dst is not None:
                T.op("pool", lambda e: e.tensor_copy(out=dstT[:, :, t * 128:(t + 1) * 128], in_=f32dst),
                     r=[F32B], w=[DSTB])

        epsc = RP.alloc([4])
        T.op("dve", lambda e: e.memset(epsc[:, 0:1], EPS), w=[CONST])
        T.op("dve", lambda e: e.memset(epsc[:, 1:2], 64.0 * EPS), w=[CONST])
        T.op("dve", lambda e: e.memset(epsc[:, 2:3], EPS / 0.64), w=[CONST])
        EPS_AP = epsc[:, 0:1]
        EPS64_AP = epsc[:, 1:2]
        EPSD_AP = epsc[:, 2:3]

        x_v = x_d.rearrange("(t p) f -> t p f", p=128)

        scope('B')
        RB = Region(156 * KB, ARENA)
        xt = [RB.alloc([D]) for _ in range(2)]
        XTB = [Buf(), Buf()]
        tmpn = [RB.alloc([D]) for _ in range(2)]
        TMPNB = [Buf(), Buf()]
        for it in range(NT + 1):
            if it in (0, 3, 6):
                issue_sec(3 + it // 3)
            if it in (2, 5, 8, 11):
                proc_sec(2 + (it - 2) // 3)
            if it < NT:
                sl = it % 2
                T.dma(xt[sl], x_v[it], w=[XTB[sl]])
                nt_stats(it, xt[sl], [XTB[sl]], tmpn[sl], TMPNB[sl])
            if it >= 1:
                t = it - 1
                nt_trans(t, tmpn[t % 2], TMPNB[t % 2], a1c, sh1c, hT, HTB)
        T.op("dve", lambda e: e.scalar_tensor_tensor(out=a2c, in0=modc[:, 24:32], scalar=1.0, in1=sm("g2"), op0=ALU.add, op1=ALU.mult),
             r=[MC[2], MC[3], SMB], w=[MODB])
        win_v = win_d.rearrange("(kc p) f -> p kc f", p=128)

        class CProj:
            def __init__(self, R, R2=None):
                R2 = R if R2 is None else R2
                w0_ = R.alloc([8, 512], BF16)
                self.stg = [R.alloc([512]) for _ in range(4)]
                self.wsec = [w0_, R2.alloc([8, 512], BF16)]
                self.WB = [Buf(), Buf()]
                self.SB = [Buf() for _ in range(4)]
                R = R2
                self.sq = [R.alloc([512]) for _ in range(3)]
                self.SQB = [Buf() for _ in range(3)]
                self.qn = [R.alloc([512], BF16) for _ in range(3)]
                self.QNB = [Buf() for _ in range(3)]
                self.nsec = 0
                self.npiece = 0

            def prefetch(self, col0):
                i = self.nsec % 2
                self.nsec += 1
                for k in range(8):
                    p = self.npiece % 4
                    self.npiece += 1
                    T.dma(self.stg[p], win_v[:, k, col0:col0 + 512], w=[self.SB[p]])
                    T.op("pool", lambda e, k=k, p=p: e.tensor_copy(out=self.wsec[i][:, k, :], in_=self.stg[p]),
                         r=[self.SB[p]], w=[self.WB[i]])
                return i

            def qk_fns(self, wi, gname, is_q, dstT, DSTB):
                wsec, WB = self.wsec[wi], self.WB[wi]
                gt = sm(gname)

                def mm(t, gi):
                    bank = gi % 3
                    for k in range(8):
                        T.op("pe", lambda e, k=k: e.matmul(psum[:, bank, :], lhsT=hT[:, k, t * 128:(t + 1) * 128], rhs=wsec[:, k, :],
                                                           start=(k == 0), stop=(k == 7)), r=[HTB, WB], w=[PB[bank]], sig=(k == 7))

                def chain(t, gi):
                    bank = gi % 3
                    s3 = gi % 3
                    sq, SQB, qn, QNB = self.sq[s3], self.SQB[s3], self.qn[s3], self.QNB[s3]
                    so = 8 + 16 * (gi % 2)
                    T.op("act", lambda e: e.activation(out=sq, in_=psum[:, bank, :], func=AF.Square), r=[PB[bank]], w=[SQB])
                    T.op("dve", lambda e: e.tensor_reduce(out=stat[:, so:so + 8], in_=sq.rearrange("p (a b) -> p a b", a=8),
                                                          axis=AX.X, op=ALU.add), r=[SQB], w=[MODB])
                    if is_q:
                        T.op("act", lambda e: e.activation(out=stat[:, so + 8:so + 16], in_=stat[:, so:so + 8], func=AF.Sqrt,
                                                           scale=1.0, bias=EPS64_AP), r=[MODB, CONST], w=[MODB])
                    else:
                        T.op("act", lambda e: e.activation(out=stat[:, so + 8:so + 16], in_=stat[:, so:so + 8], func=AF.Sqrt,
                                                           scale=1.0 / 64, bias=EPS_AP), r=[MODB, CONST], w=[MODB])
                    T.op("dve", lambda e: e.reciprocal(out=stat[:, so:so + 8], in_=stat[:, so + 8:so + 16]), r=[MODB], w=[MODB])
                    T.op("dve", lambda e: e.tensor_tensor(
                        out=sq.rearrange("p (a b) -> p a b", a=8), in0=psum[:, bank, :].rearrange("p (a b) -> p a b", a=8),
                        in1=stat[:, so:so + 8].unsqueeze(2).to_broadcast([128, 8, 64]), op=ALU.mult),
                        r=[PB[bank], MODB], w=[SQB])
                    T.op("pool", lambda e: e.tensor_tensor(
                        out=qn.rearrange("p (a b) -> p a b", a=8), in0=sq.rearrange("p (a b) -> p a b", a=8),
                        in1=gt.unsqueeze(1).to_broadcast([128, 8, 64]), op=ALU.mult), r=[SQB, SMB], w=[QNB])

                def trans(t, gi):
                    s3 = gi % 3
                    tb_i = 3 + (gi % 2)
                    tb = psum[:, tb_i, :].bitcast(BF16)
                    for j in range(4):
                        T.op("pe", lambda e, j=j: e.transpose(out=tb[:, j * 128:(j + 1) * 128], in_=self.qn[s3][:, j * 128:(j + 1) * 128],
                                                              identity=ident_bf), r=[self.QNB[s3], CONST], w=[PB[tb_i]], sig=(j == 3))
                    T.op("act", lambda e: e.activation(
                        out=dstT[:, :, t * 128:(t + 1) * 128], in_=tb[:, 0:512].rearrange("p (a b) -> p a b", a=4), func=AF.Copy),
                        r=[PB[tb_i]], w=[DSTB])

                return mm, chain, trans

            def run(self, fns_list, hook=None):
                items = [(s_, t) for s_ in range(len(fns_list)) for t in range(NT)]
                n = len(items)
                for it in range(n + 3):
                    if it == NT and hook is not None:
                        hook()
                    if it < n:
                        s_, t = items[it]
                        fns_list[s_][0](t, it)
                    if 1 <= it < n + 1:
                        s_, t = items[it - 1]
                        fns_list[s_][1](t, it - 1)
                    if it >= 3:
                        s_, t = items[it - 3]
                        fns_list[s_][2](t, it - 3)

        cpA = CProj(Region(190 * KB, ARENA), Region(172 * KB, 190 * KB))
        wA0 = cpA.prefetch(0)
        T.barrier()
        if debug:
            T.dma(dbg_hT, hT, r=[HTB])
            T.dma(dbg_sm, arena[:, 0:28 * KB // 4], r=[SMB, MODB, CONST])
            T.barrier()

        scope('CA')
        qTa = carve(76 * KB, [4, S], BF16)
        kTa = carve(156 * KB, [4, S], BF16)
        Va = [carve(ACC0 + p * 16 * KB, [NT, 512], BF16) for p in range(3)]
        QTAB, KTAB, VAB = Buf(), Buf(), Buf()
        cp = cpA
        w0 = wA0
        w1 = cp.prefetch(512)
        w2box = []
        cp.run([cp.qk_fns(w0, "gqa", True, qTa, QTAB), cp.qk_fns(w1, "gka", False, kTa, KTAB)],
               hook=lambda: w2box.append(cp.prefetch(1024)))
        w2 = w2box[0]
        wsec, WB = cp.wsec[w2], cp.WB[w2]

        def tok_cols(p, blk):
            if p == 0:
                return slice(blk * 128, (blk + 1) * 128)
            if p == 1:
                r4, n = blk // 4, blk % 4
                return slice(512 * n + r4, 512 * n + 512, 4)
            return slice(blk, S, 16)

        vi = 0
        for p in range(3):
            for blk in range(NT):
                pb_i = vi % 3
                cols = tok_cols(p, blk)
                for k in range(8):
                    T.op("pe", lambda e, k=k: e.matmul(psum[:, pb_i, :], lhsT=hT[:, k, cols], rhs=wsec[:, k, :],
                                                       start=(k == 0), stop=(k == 7)), r=[HTB, WB], w=[PB[pb_i]], sig=(k == 7))
                if vi % 2 == 0:
                    T.op("act", lambda e: e.activation(out=Va[p][:, blk, :], in_=psum[:, pb_i, :], func=AF.Copy), r=[PB[pb_i]], w=[VAB])
                else:
                    T.op("dve", lambda e: e.tensor_copy(out=Va[p][:, blk, :], in_=psum[:, pb_i, :]), r=[PB[pb_i]], w=[VAB])
                vi += 1
        T.barrier()
        if debug:
            T.dma(dbg_qk[:, 0], qTa, r=[QTAB])
            T.dma(dbg_qk[:, 1], kTa, r=[KTAB])
            T.barrier()

        scope('D')
        RD = Region(172 * KB, ARENA)
        biasr = [RD.alloc([5, 128]) for _ in range(2)]
        BIASB = [Buf(), Buf()]
        stmp = [RD.alloc([512]) for _ in range(2)]
        STB = [Buf(), Buf()]
        ptl = [RD.alloc([512], BF16) for _ in range(2)]
        PTB = [Buf(), Buf()]
        rtl = [RD.alloc([512]) for _ in range(2)]
        RTB = [Buf(), Buf()]
        OB = [PB[4], PB[5], PB[6], PB[7]]

        def head_units(h):
            units = []
            for p in range(3):
                nb = [16, 4, 1][p]
                ngrp = NT // nb
                for prev in (0, 1):
                    if p == 2 and prev:
                        continue
                    tiles = []
                    for grp in range(ngrp):
                        for n in range(nb):
                            if prev and n == 0:
                                continue
                            blk = grp * nb + n
                            qc = tok_cols(p, blk)
                            kc = tok_cols(p, blk - 1) if prev else qc
                            if p == 0:
                                outs = [(slice(0, 128), blk // 4, slice((blk % 4) * 128, (blk % 4) * 128 + 128))]
                            elif p == 1:
                                outs = [(slice(0, 128), n, slice(grp, 512, 4))]
                            else:
                                outs = [(slice(32 * g, 32 * g + 32), g, slice(blk, 512, 16)) for g in range(4)]
                            tiles.append((kc, qc, p, blk - 1 if prev else blk, outs))
                    bidx = [0, 2, 4][p] + prev
                    for i in range(0, len(tiles), 4):
                        units.append((bidx, tiles[i:i + 4]))
            return units

        allu = []
        for h in range(8):
            hu = head_units(h)
            for i, u in enumerate(hu):
                allu.append((h, i == 0, i == len(hu) - 1, u))
        started = {}

        def emit_bias(h):
            bs = biasr[h % 2]
            for bi, (src, dil) in enumerate([("dcur", 1), ("dprev", 1), ("dcur", 4), ("dprev", 4), ("dcur", 16)]):
                T.op("pool", lambda e, bi=bi, src=src, dil=dil: e.tensor_scalar(
                    out=bs[:, bi, :], in0=sm(src), scalar1=-SLOPES_A[h] * dil, scalar2=None, op0=ALU.mult),
                    r=[SMB], w=[BIASB[h % 2]])

        def emit_qk(g):
            h, first, last_u, (bidx, tiles) = allu[g]
            j, hh = h // 2, h % 2
            if first:
                emit_bias(h)
            sb_i = g % 4
            nt_ = len(tiles)
            for i, (kc, qc, p, vblk, outs) in enumerate(tiles):
                T.op("pe", lambda e, i=i, kc=kc, qc=qc: e.matmul(
                    psum[:, sb_i, i * 128:(i + 1) * 128], lhsT=kTa[64 * hh:64 * hh + 64, j, kc], rhs=qTa[64 * hh:64 * hh + 64, j, qc],
                    start=True, stop=True), r=[KTAB, QTAB], w=[PB[sb_i]], sig=(i == nt_ - 1))

        def emit_rest(g):
            h, first, last_u, (bidx, tiles) = allu[g]
            j, hh = h // 2, h % 2
            nlo, dlo = (0, 64) if hh == 0 else (64, 0)
            bs = biasr[h % 2]
            sb_i = g % 4
            s2 = g % 2
            nt_ = len(tiles)
            T.op("dve", lambda e: e.tensor_tensor(
                out=stmp[s2][:, 0:nt_ * 128].rearrange("p (a b) -> p a b", a=nt_),
                in0=psum[:, sb_i, 0:nt_ * 128].rearrange("p (a b) -> p a b", a=nt_),
                in1=bs[:, bidx, :].unsqueeze(1).to_broadcast([128, nt_, 128]), op=ALU.add),
                r=[PB[sb_i], BIASB[h % 2]], w=[STB[s2]])
            T.op("act", lambda e: e.activation(out=ptl[s2][:, 0:nt_ * 128], in_=stmp[s2][:, 0:nt_ * 128], func=AF.Exp),
                 r=[STB[s2]], w=[PTB[s2]])

        def emit_pv(g):
            h, first, last_u, (bidx, tiles) = allu[g]
            j, hh = h // 2, h % 2
            nlo, dlo = (0, 64) if hh == 0 else (64, 0)
            s2 = g % 2
            nt_ = len(tiles)
            if first:
                started.clear()
            for i, (kc, qc, p, vblk, outs) in enumerate(tiles):
                for oi, (msl, bank, ocols) in enumerate(outs):
                    last = (i == nt_ - 1 and oi == len(outs) - 1)
                    mv = ptl[s2][:, i * 128:(i + 1) * 128][:, msl]
                    st_n = (bank, 0) not in started
                    started[(bank, 0)] = 1
                    T.op("pe", lambda e, mv=mv, bank=bank, ocols=ocols, st_n=st_n, p=p, vblk=vblk: e.matmul(
                        psum[nlo:nlo + 64, 4 + bank, ocols], lhsT=Va[p][:, vblk, h * 64:(h + 1) * 64], rhs=mv,
                        start=st_n, stop=True, skip_group_check=True), r=[PTB[s2], VAB], w=[OB[bank]], sig=False)
                    st_d = (bank, 1) not in started
                    started[(bank, 1)] = 1
                    T.op("pe", lambda e, mv=mv, bank=bank, ocols=ocols, st_d=st_d: e.matmul(
                        psum[dlo:dlo + 64, 4 + bank, ocols], lhsT=ones_bf, rhs=mv,
                        start=st_d, stop=True, skip_group_check=True), r=[PTB[s2], CONST], w=[OB[bank]], sig=last)
            if last_u:
                for bank in range(4):
                    r2 = bank % 2
                    T.op("act", lambda e, bank=bank, r2=r2: e.activation(out=rtl[r2][dlo:dlo + 64, :], in_=psum[dlo:dlo + 64, 4 + bank, :],
                                                                       func=AF.Ln), r=[OB[bank]], w=[RTB[r2]])
                    T.op("act", lambda e, bank=bank, r2=r2: e.activation(out=rtl[r2][dlo:dlo + 64, :], in_=rtl[r2][dlo:dlo + 64, :],
                                                                       func=AF.Exp, scale=-1.0), r=[RTB[r2]], w=[RTB[r2]])
                    T.op("dve", lambda e, bank=bank, r2=r2: e.tensor_tensor(
                        out=catT[nlo:nlo + 64, j, bank * 512:(bank + 1) * 512], in0=psum[nlo:nlo + 64, 4 + bank, :],
                        in1=rtl[r2][dlo:dlo + 64, :], op=ALU.mult), r=[OB[bank], RTB[r2]], w=[CATB])

        emit_qk(0)
        emit_qk(1)
        emit_rest(0)
        for g in range(len(allu)):
            if g + 2 < len(allu):
                emit_qk(g + 2)
            if g + 1 < len(allu):
                emit_rest(g + 1)
            emit_pv(g)
        cpB = CProj(Region(187 * KB, ARENA), Region(156 * KB, 187 * KB))
        wB0 = cpB.prefetch(1536)
        T.barrier()

        scope('CB')
        qTb = carve(ACC0, [4, S], BF16)
        kTb = carve(ACC0 + 16 * KB, [4, S], BF16)
        Vb = carve(ACC0 + 32 * KB, [NT, 4 * 130], BF16)
        QTBB, KTBB, VBB = Buf(), Buf(), Buf()
        RC = Region(156 * KB, ARENA)
        cp = cpB
        w0 = wB0
        w1 = cp.prefetch(2048)
        w2box = []
        cp.run([cp.qk_fns(w0, "gqb", True, qTb, QTBB), cp.qk_fns(w1, "gkb", False, kTb, KTBB)],
               hook=lambda: w2box.append(cp.prefetch(2560)))
        w2 = w2box[0]
        wsec, WB = cp.wsec[w2], cp.WB[w2]
        Vb4 = Vb.rearrange("p t (h c) -> p t h c", h=4)
        T.op("pool", lambda e: e.memset(Vb, 1.0), w=[VBB])
        for t in range(NT):
            pb_i = t % 3
            for k in range(8):
                T.op("pe", lambda e, k=k: e.matmul(psum[:, pb_i, :], lhsT=hT[:, k, t * 128:(t + 1) * 128], rhs=wsec[:, k, :],
                                                   start=(k == 0), stop=(k == 7)), r=[HTB, WB], w=[PB[pb_i]], sig=(k == 7))
            T.op("act", lambda e: e.activation(out=Vb4[:, t, :, 0:128], in_=psum[:, pb_i, :].rearrange("p (h c) -> p h c", h=4),
                                               func=AF.Copy), r=[PB[pb_i]], w=[VBB])
        T.barrier()
        if debug:
            T.dma(dbg_qk[:, 2], qTb, r=[QTBB])
            T.dma(dbg_qk[:, 3], kTb, r=[KTBB])
            T.barrier()

        scope('E')
        RE = Region(156 * KB, ARENA)
        Pt_a = [RE.alloc([NT, 512], BF16) for _ in range(2)]
        Pt_b = [carve(HT0 + m_ * 16 * KB, [NT, 512], BF16) for m_ in range(2)]
        PtS = [Pt_a, Pt_b]
        PTBS = [[[Buf() for _ in range(NT)] for _ in range(2)] for _ in range(2)]
        Rw = [RE.alloc([4, 2, 130]) for _ in range(2)]
        RWB = [Buf(), Buf()]
        osb = [RE.alloc([128]) for _ in range(4)]
        OSB = [Buf() for _ in range(4)]
        onb2 = [RE.alloc([4, 128], BF16) for _ in range(2)]
        ONB2 = [Buf(), Buf()]
        junk = RE.alloc([128])
        JB = Buf()
        est = RE.alloc([32])
        ESB = Buf()
        bcol = sm("bcolb").rearrange("p (h d) -> p h d", h=4)
        gdiff = sm("gdiff")
        SCB = [0, 1, 6, 7]
        OUTB = [2, 3, 4]
        cnt = {"s": 0, "o": 0}
        chunks = [(h, Q) for h in range(4) for Q in range(4)]

        def emit_U(ci, m):
            h, Q = chunks[ci]
            Pt = PtS[ci % 2]
            PTB2 = PTBS[ci % 2]
            nkb = 4 * Q + 4
            for kb in range(nkb):
                q0 = max(512 * Q, 128 * kb)
                n = 512 * Q + 512 - q0
                lo = q0 - 512 * Q
                sb_i = SCB[cnt["s"] % 4]
                cnt["s"] += 1
                T.op("pe", lambda e: e.matmul(
                    psum[:, sb_i, 0:n], lhsT=kTb[64 * m:64 * m + 64, h, kb * 128:(kb + 1) * 128],
                    rhs=qTb[64 * m:64 * m + 64, h, q0:q0 + n], start=True, stop=True),
                    r=[KTBB, QTBB], w=[PB[sb_i]])
                d = kb - 4 * Q + 15
                T.op("act", lambda e: e.activation(
                    out=Pt[m][:, kb, lo:lo + n], in_=psum[:, sb_i, 0:n], func=AF.Exp, bias=bcol[:, h, d:d + 1]),
                    r=[PB[sb_i], SMB], w=[PTB2[m][kb]])
                if kb >= 4 * Q:
                    T.op("pool", lambda e: e.tensor_tensor(
                        out=Pt[m][:, kb, lo:lo + 128], in0=Pt[m][:, kb, lo:lo + 128], in1=tri_bf, op=ALU.mult),
                        r=[PTB2[m][kb], CONST], w=[PTB2[m][kb]])

        def emit_V(ci, m):
            h, Q = chunks[ci]
            Pt = PtS[ci % 2]
            PTB2 = PTBS[ci % 2]
            cp_ = ci % 2
            for qq in range(4):
                qb = 4 * Q + qq
                ob_i = OUTB[cnt["o"] % 3]
                cnt["o"] += 1
                for kb in range(qb + 1):
                    T.op("pe", lambda e, kb=kb: e.matmul(
                        psum[:, ob_i, 0:129], lhsT=Pt[m][:, kb, qq * 128:(qq + 1) * 128],
                        rhs=Vb4[:, kb, h, 0:129], start=(kb == 0), stop=(kb == qb)),
                        r=[PTB2[m][kb], VBB], w=[PB[ob_i]], sig=(kb == qb))
                T.op("dve", lambda e: e.tensor_copy(out=Rw[cp_][:, qq, m, 0:129], in_=psum[:, ob_i, 0:129]),
                     r=[PB[ob_i]], w=[RWB[cp_]])

        def emit_post(ci):
            h, Q = chunks[ci]
            cp_ = ci % 2
            R = Rw[cp_]
            RB = RWB[cp_]
            so = 16 * cp_
            T.op("dve", lambda e: e.reciprocal(out=est[:, so:so + 8].rearrange("p (q m) -> p q m", q=4), in_=R[:, :, :, 128]),
                 r=[RB], w=[ESB])
            for qq in range(4):
                o4 = qq
                T.op("dve", lambda e: e.tensor_tensor(out=est[:, so + 8 + qq:so + 9 + qq], in0=est[:, so + 2 * qq + 1:so + 2 * qq + 2],
                                                      in1=neglam, op=ALU.mult), r=[ESB, MODB], w=[ESB])
                T.op("dve", lambda e: e.tensor_scalar(out=osb[o4], in0=R[:, qq, 0, 0:128], scalar1=est[:, so + 2 * qq:so + 2 * qq + 1],
                                                      scalar2=None, op0=ALU.mult), r=[RB, ESB], w=[OSB[o4]])
                T.op("dve", lambda e: e.scalar_tensor_tensor(out=osb[o4], in0=R[:, qq, 1, 0:128], scalar=est[:, so + 8 + qq:so + 9 + qq],
                                                             in1=osb[o4], op0=ALU.mult, op1=ALU.add), r=[RB, ESB, OSB[o4]], w=[OSB[o4]])
                T.op("dve", lambda e: e.scalar_tensor_tensor(out=junk, in0=osb[o4], scalar=1.0, in1=osb[o4], op0=ALU.mult, op1=ALU.mult,
                                                             accum_out=est[:, so + 12 + qq:so + 13 + qq]), r=[OSB[o4]], w=[JB, ESB])
            T.op("act", lambda e: e.activation(out=est[:, so + 12:so + 16], in_=est[:, so + 12:so + 16], func=AF.Ln,
                                               scale=1.0 / (128 * 0.64), bias=EPSD_AP), r=[ESB, CONST], w=[ESB])
            T.op("act", lambda e: e.activation(out=est[:, so + 12:so + 16], in_=est[:, so + 12:so + 16], func=AF.Exp, scale=-0.5),
                 r=[ESB], w=[ESB])
            onb, ONB = onb2[cp_], ONB2[cp_]
            for qq in range(4):
                T.op("dve", lambda e: e.scalar_tensor_tensor(out=onb[:, qq, :], in0=osb[qq], scalar=est[:, so + 12 + qq:so + 13 + qq],
                                                             in1=gdiff, op0=ALU.mult, op1=ALU.mult), r=[OSB[qq], ESB, SMB], w=[ONB])

        def emit_post_b(ci):
            h, Q = chunks[ci]
            cp_ = ci % 2
            onb, ONB = onb2[cp_], ONB2[cp_]
            tb = psum[:, 5, :].bitcast(BF16)
            for qq in range(4):
                T.op("pe", lambda e: e.transpose(out=tb[:, qq * 128:(qq + 1) * 128], in_=onb[:, qq, :], identity=ident_bf),
                     r=[ONB, CONST], w=[PB[5]], sig=(qq == 3))
            T.op("act", lambda e: e.activation(out=catT[:, 4 + h, Q * 512:(Q + 1) * 512], in_=tb[:, 0:512], func=AF.Copy),
                 r=[PB[5]], w=[CATB])

        emit_U(0, 0)
        emit_U(0, 1)
        for ci in range(len(chunks)):
            nxt = ci + 1 < len(chunks)
            emit_V(ci, 0)
            if nxt:
                emit_U(ci + 1, 0)
            if ci >= 1:
                emit_post_b(ci - 1)
            emit_V(ci, 1)
            if nxt:
                emit_U(ci + 1, 1)
            emit_post(ci)
        emit_post_b(len(chunks) - 1)
        T.barrier()
        if debug:
            T.dma(dbg_catT, catT, r=[CATB])
            T.barrier()

        scope('F')
        RF = Region(156 * KB, ARENA)
        wo = RF.alloc([8, D], BF16)
        WOB = Buf()
        wos = [RF.alloc([2, D]) for _ in range(2)]
        WOSB = [Buf(), Buf()]
        xt = [RF.alloc([D]) for _ in range(2)]
        XTB = [Buf(), Buf()]
        wout_v = wout_d.rearrange("(kc p) f -> p kc f", p=128)
        for pc in range(4):
            sl = pc % 2
            T.dma(wos[sl], wout_v[:, 2 * pc:2 * pc + 2, :], w=[WOSB[sl]])
            T.op("dve", lambda e: e.tensor_tensor(out=wo[:, 2 * pc:2 * pc + 2, :], in0=wos[sl],
                                                  in1=GB1.unsqueeze(1).to_broadcast([128, 2, D]), op=ALU.mult),
                 r=[WOSB[sl], SMB], w=[WOB])
        ev_cast = (("c", "dve"), T.cnt["dve"])

        def f_tile(t):
            sl = t % 2
            T.dma(xt[sl], x_v[t], w=[XTB[sl]])
            for half in range(2):
                pb_i = 6 + half
                for k in range(8):
                    T.op("pe", lambda e, k=k: e.matmul(psum[:, pb_i, :], lhsT=catT[:, k, t * 128:(t + 1) * 128],
                                                       rhs=wo[:, k, half * 512:(half + 1) * 512], start=(k == 0), stop=(k == 7)),
                         r=[CATB, WOB], w=[PB[pb_i]], sig=(k == 7))
                T.op("dve", lambda e: e.tensor_tensor(out=acc[:, t, half * 512:(half + 1) * 512], in0=psum[:, pb_i, :],
                                                      in1=xt[sl][:, half * 512:(half + 1) * 512], op=ALU.add),
                     r=[PB[pb_i], XTB[sl]], w=[ACCB[t]])

        scope('G')
        RGa = Region(172 * KB, 188 * KB)
        RG = Region(196 * KB, ARENA)
        tmpn = [RGa.alloc([D]) for _ in range(2)]
        TMPNB = [Buf(), Buf()]
        h2f = [RGa.alloc([8, 128]) for _ in range(2)]
        H2FB = [Buf(), Buf()]
        for b_ in TMPNB + H2FB:
            b_.w = ev_cast
        lg = [RG.alloc([NE]) for _ in range(2)]
        LGB = [Buf(), Buf()]
        m8 = [RG.alloc([8]) for _ in range(2)]
        ex = [RG.alloc([NE]) for _ in range(2)]
        wr = sm("wr").rearrange("p (k e) -> p k e", k=8)
        GAB = Buf()
        GTB = Buf()

        Lg = RG.alloc([NT, NE])
        Wg = RG.alloc([NT, NE])
        Eq = RG.alloc([NT, NE])
        mt = RG.alloc([4, NT])
        LGALL = Buf()

        def g_router(t):
            s2 = t % 2
            for k in range(8):
                T.op("pe", lambda e, k=k: e.matmul(psum[:, 4 + s2, 0:NE], lhsT=h2f[s2][:, k, :], rhs=wr[:, k, :], start=(k == 0), stop=(k == 7)),
                     r=[H2FB[s2], SMB], w=[PB[4 + s2]], sig=(k == 7))
            T.op("dve", lambda e: e.tensor_tensor(out=Lg[:, t, :], in0=psum[:, 4 + s2, 0:NE], in1=sm("br"), op=ALU.add),
                 r=[PB[4 + s2], SMB], w=[LGALL])

        for it in range(NT + 3):
            if it < NT:
                f_tile(it)
            if 1 <= it < NT + 1:
                t = it - 1
                nt_stats(t, acc[:, t, :], [ACCB[t]], tmpn[t % 2], TMPNB[t % 2])
            if 2 <= it < NT + 2:
                t = it - 2
                nt_trans(t, tmpn[t % 2], TMPNB[t % 2], a2c, sh2c, hT, HTB, f32dst=h2f[t % 2], F32B=H2FB[t % 2])
            if it >= 3:
                g_router(it - 3)
        if debug:
            T.barrier()
            T.dma(dbg_x1, acc, r=ACCB)
            T.barrier()
        def bc3(ap2):
            return ap2.unsqueeze(2).to_broadcast([128, NT, NE])
        for r_ in range(4):
            src = Lg if r_ == 0 else Wg
            T.op("dve", lambda e: e.tensor_reduce(out=mt[:, r_, :], in_=src, axis=AX.X, op=ALU.max), r=[LGALL], w=[LGALL])
            if r_ < 3:
                T.op("dve", lambda e: e.tensor_tensor(out=Eq, in0=src, in1=bc3(mt[:, r_, :]), op=ALU.is_ge), r=[LGALL], w=[LGALL])
                T.op("dve", lambda e: e.scalar_tensor_tensor(out=Wg, in0=Eq, scalar=-1.0e9, in1=src, op0=ALU.mult, op1=ALU.add),
                     r=[LGALL], w=[LGALL])
        T.op("dve", lambda e: e.tensor_tensor(out=Eq, in0=Lg, in1=bc3(mt[:, 3, :]), op=ALU.is_ge), r=[LGALL], w=[LGALL])
        T.op("dve", lambda e: e.tensor_tensor(out=Wg, in0=Lg, in1=bc3(mt[:, 0, :]), op=ALU.subtract), r=[LGALL], w=[LGALL])
        T.op("act", lambda e: e.activation(out=Wg, in_=Wg, func=AF.Exp), r=[LGALL], w=[LGALL])
        T.op("dve", lambda e: e.tensor_tensor(out=Wg, in0=Wg, in1=Eq, op=ALU.mult), r=[LGALL], w=[LGALL])
        T.op("dve", lambda e: e.tensor_reduce(out=mt[:, 1, :], in_=Wg, axis=AX.X, op=ALU.add), r=[LGALL], w=[LGALL])
        T.op("dve", lambda e: e.reciprocal(out=mt[:, 2, :], in_=mt[:, 1, :]), r=[LGALL], w=[LGALL])
        T.op("dve", lambda e: e.tensor_tensor(out=G_all, in0=Wg, in1=bc3(mt[:, 2, :]), op=ALU.mult), r=[LGALL], w=[GAB])
        GTT = [Buf() for _ in range(NT)]

        def emit_gt(t):
            s2 = t % 2
            T.op("pe", lambda e: e.transpose(out=psum[0:NE, 6 + s2, 0:128], in_=G_all[:, t, :], identity=ident_f), r=[GAB, SMB], w=[PB[6 + s2]])
            T.op("act", lambda e: e.activation(out=GT[0:NE, t * 128:(t + 1) * 128], in_=psum[0:NE, 6 + s2, 0:128], func=AF.Copy),
                 r=[PB[6 + s2]], w=[GTT[t]])
        T.barrier()
        if debug:
            T.dma(dbg_h2T, hT, r=[HTB])
            T.barrier()

        scope('H')
        RH = Region(R30, ARENA)
        actT = RH.alloc([8, S], BF16)
        ACTB = [[Buf() for _ in range(4)] for _ in range(8)]
        wgu = [RH.alloc([8, 256], BF16) for _ in range(2)]
        WGUB = [Buf() for _ in range(2)]
        wdn = RH.alloc([8, D], BF16)
        WDB = [Buf() for _ in range(8)]
        stgd = RH.alloc([D])
        STGDB = Buf()
        stg = [RH.alloc([2 * D]) for _ in range(2)]
        STGB = [Buf(), Buf()]
        STGU = [Buf(), Buf()]
        gcb = [RH.alloc([512], BF16) for _ in range(2)]
        sgb = [RH.alloc([512], BF16) for _ in range(2)]
        aab = [RH.alloc([512], BF16) for _ in range(2)]
        GCB, SGB, AAB = [Buf(), Buf()], [Buf(), Buf()], [Buf(), Buf()]
        bgu = sm("bgu").rearrange("p (e c) -> p e c", e=NE)
        GB2K = GB1
        GKB = SMB
        T.op("dve", lambda e: e.tensor_scalar(out=GB2K, in0=GB2, scalar1=1.0 / 1.702, scalar2=None, op0=ALU.mult), r=[SMB], w=[GKB])
        bds = stg[0][0:NE, 0:D]
        T.dma(bds, bd_d, w=[STGB[0]])
        T.op("pool", lambda e: e.tensor_tensor(out=bdp[0:NE, :], in0=bds, in1=GB2[0:NE, :], op=ALU.mult), r=[STGB[0], SMB], w=[GTB])
        def emit_bias_term(t0, t1):
            for t in range(t0, t1):
                for half in range(2):
                    pb_i = 4 + (2 * t + half) % 4
                    T.op("pe", lambda e: e.matmul(psum[:, pb_i, :], lhsT=GT[0:NE, t * 128:(t + 1) * 128], rhs=bdp[0:NE, half * 512:(half + 1) * 512],
                                                  start=True, stop=True), r=[GTB, GTT[t]], w=[PB[pb_i]])
                    T.op("dve", lambda e: e.tensor_tensor(out=acc[:, t, half * 512:(half + 1) * 512], in0=psum[:, pb_i, :],
                                                          in1=acc[:, t, half * 512:(half + 1) * 512], op=ALU.add),
                         r=[PB[pb_i], ACCB[t]], w=[ACCB[t]])
        def gu_prefetch(q):
            ex_q, ffp_q = q // 8, q % 8
            wgu_q = wgu_d[ex_q].rearrange("(kc p) f -> p kc f", p=128)
            ss = q % 2
            sv = stg[ss].rearrange("p (k u f) -> p k u f", k=8, u=2)
            T.dma(sv[:, :, 0, :], wgu_q[:, :, ffp_q * 128:(ffp_q + 1) * 128], w=[STGB[ss]])
            T.dma(sv[:, :, 1, :], wgu_q[:, :, D + ffp_q * 128:D + (ffp_q + 1) * 128], w=[STGU[ss]])
            T.op("act", lambda e: e.activation(out=wgu[q % 2], in_=stg[ss].rearrange("p (k c) -> p k c", k=8), func=AF.Copy),
                 r=[STGB[ss], STGU[ss]], w=[WGUB[q % 2]])

        ui = 0
        gu_prefetch(0)
        for ex_i in range(NE):
            wd_e = wd_d[ex_i].rearrange("(kc p) f -> p kc f", p=128)
            for ffp in range(8):
                q = ex_i * 8 + ffp
                if q + 1 < NE * 8:
                    gu_prefetch(q + 1)
                T.dma(stgd, wd_e[:, ffp, :], w=[STGDB])
                gs = q % 2
                for tc in range(4):
                    u2 = ui % 2
                    ui += 1
                    pg, pu = 2 * u2, 2 * u2 + 1
                    for k in range(8):
                        T.op("pe", lambda e, k=k: e.matmul(psum[:, pg, :], lhsT=wgu[gs][:, k, 0:128], rhs=hT[:, k, tc * 512:(tc + 1) * 512],
                                                           start=(k == 0), stop=(k == 7)), r=[WGUB[gs], HTB], w=[PB[pg]], sig=(k == 7))
                    for k in range(8):
                        T.op("pe", lambda e, k=k: e.matmul(psum[:, pu, :], lhsT=wgu[gs][:, k, 128:256], rhs=hT[:, k, tc * 512:(tc + 1) * 512],
                                                           start=(k == 0), stop=(k == 7)), r=[WGUB[gs], HTB], w=[PB[pu]], sig=(k == 7))
                    T.op("dve", lambda e: e.tensor_scalar(out=gcb[u2], in0=psum[:, pg, :], scalar1=bgu[:, ex_i, ffp:ffp + 1], scalar2=7.0,
                                                          op0=ALU.add, op1=ALU.min), r=[PB[pg], SMB], w=[GCB[u2]])
                    T.op("act", lambda e: e.activation(out=sgb[u2], in_=gcb[u2], func=AF.Silu, scale=1.702), r=[GCB[u2]], w=[SGB[u2]])
                    T.op("dve", lambda e: e.tensor_scalar(out=aab[u2], in0=psum[:, pu, :], scalar1=bgu[:, ex_i, 8 + ffp:9 + ffp], scalar2=-7.0,
                                                          op0=ALU.add, op1=ALU.max), r=[PB[pu], SMB], w=[AAB[u2]])
                    T.op("dve", lambda e: e.tensor_scalar(out=aab[u2], in0=aab[u2], scalar1=7.0, scalar2=1.0, op0=ALU.min, op1=ALU.add),
                         r=[AAB[u2]], w=[AAB[u2]])
                    T.op("pool", lambda e: e.tensor_tensor(out=actT[:, ffp, tc * 512:(tc + 1) * 512], in0=sgb[u2], in1=aab[u2], op=ALU.mult),
                         r=[SGB[u2], AAB[u2]], w=[ACTB[ffp][tc]])
                T.op("dve", lambda e: e.tensor_tensor(out=wdn[:, ffp, :], in0=stgd, in1=GB2K, op=ALU.mult),
                     r=[STGDB, GKB], w=[WDB[ffp]])
                if ex_i == 0:
                    emit_gt(2 * ffp)
                    emit_gt(2 * ffp + 1)
                    emit_bias_term(2 * ffp, 2 * ffp + 2)
            for t in range(NT):
                for half in range(2):
                    pb_i = 4 + (2 * t + half) % 4
                    for f in range(8):
                        T.op("pe", lambda e, f=f: e.matmul(psum[:, pb_i, :], lhsT=actT[:, f, t * 128:(t + 1) * 128],
                                                           rhs=wdn[:, f, half * 512:(half + 1) * 512], start=(f == 0), stop=(f == 7)),
                             r=[ACTB[f][t // 4], WDB[f]], w=[PB[pb_i]], sig=(f == 7))
                    T.op("dve", lambda e: e.scalar_tensor_tensor(
                        out=acc[:, t, half * 512:(half + 1) * 512], in0=psum[:, pb_i, :], scalar=G_all[:, t, ex_i:ex_i + 1],
                        in1=acc[:, t, half * 512:(half + 1) * 512], op0=ALU.mult, op1=ALU.add),
                        r=[PB[pb_i], GAB, ACCB[t]], w=[ACCB[t]])
        scope('OUT')
        out_v = out_d.rearrange("(t p) f -> t p f", p=128)
        for t in range(NT):
            T.dma(out_v[t], acc[:, t, :], r=[ACCB[t]])
        T.barrier()
        scope(None)
    return nc


def _prep_smalls(inp, b):
    sm = np.zeros((128, NS), np.float32)

    def put(name, arr):
        o, w = _SM[name]
        arr = np.asarray(arr, np.float32)
        assert arr.shape == (128, w), (name, arr.shape)
        sm[:, o:o + w] = arr

    def cols(v, n):
        return np.asarray(v, np.float32).reshape(n, 128).T

    def bc(v):
        v = np.asarray(v, np.float32).reshape(1, -1)
        return np.broadcast_to(v, (128, v.shape[1]))

    put("c", cols(inp["c"][b], 8))
    put("bada", cols(inp["b_ada"][0], 48))
    put("g1", cols(inp["norm1_g"][0], 8))
    put("g2", cols(inp["norm2_g"][0], 8))
    put("gqa", bc(inp["a_q_norm_g"][0]))
    put("gka", bc(inp["a_k_norm_g"][0]))
    put("gqb", bc(inp["b_q_norm_g"][0]))
    put("gkb", bc(inp["b_k_norm_g"][0]))
    put("lam", bc(np.concatenate([inp["lambda_q1"][0], inp["lambda_k1"][0], inp["lambda_q2"][0], inp["lambda_k2"][0]])))
    put("gdiff", bc(inp["diff_norm_g"][0]))
    put("br", bc(inp["b_router"][0]))
    put("bgu", np.asarray(inp["b_gate_up"][0], np.float32).reshape(NE, 16, 128).transpose(2, 0, 1).reshape(128, NE * 16))
    put("wr", np.asarray(inp["w_router"][0], np.float32).reshape(8, 128, NE).transpose(1, 0, 2).reshape(128, 8 * NE))
    put("bg1", bc(inp["b_ada"][0][2 * D:3 * D]))
    put("bg2", bc(inp["b_ada"][0][5 * D:6 * D]))
    jj = np.arange(128)[:, None].astype(np.float64)
    ii = np.arange(128)[None, :].astype(np.float64)
    put("ident", np.eye(128))
    put("dcur", np.where(ii >= jj, ii - jj, BIG))
    put("dprev", np.where(jj >= ii, 128 + ii - jj, BIG))
    put("tri", (jj <= ii).astype(np.float32))
    bcol = np.zeros((128, 4, 19))
    for h in range(4):
        for d in range(19):
            bcol[:, h, d] = SLOPES_B[h] * (128.0 * (d - 15) + np.arange(128) - 256.0)
    put("bcolb", bcol.reshape(128, 76))
    return sm


_NC_CACHE = {}


def kernel(**inputs):
    inp = {k: np.asarray(v) for k, v in inputs.items()}
    if "nc" not in _NC_CACHE:
        _NC_CACHE["nc"] = build_nc()
    nc = _NC_CACHE["nc"]
    shared = {
        "w_ada": np.ascontiguousarray(inp["w_ada"][0], np.float32),
        "w_in": np.ascontiguousarray(inp["w_in"][0], np.float32),
        "w_out": np.ascontiguousarray(inp["w_out"][0], np.float32),
        "w_gate_up": np.ascontiguousarray(inp["w_gate_up"][0], np.float32),
        "w_down": np.ascontiguousarray(inp["w_down"][0], np.float32),
        "b_down": np.ascontiguousarray(inp["b_down"][0], np.float32),
    }
    in_maps = []
    for b in range(8):
        m = dict(shared)
        m["x"] = np.ascontiguousarray(inp["x"][b], np.float32)
        m["smalls"] = _prep_smalls(inp, b)
        in_maps.append(m)
    res = run_bass_kernel_spmd(nc, in_maps, core_ids=list(range(8)))
    return np.stack([np.asarray(r["out"], np.float32) for r in res.results], axis=0)
```
